# Optimizing a Trainium2 kernel written in Bass

```python
import math
import jax, jax.numpy as jnp
from jax import lax
import numpy as np

D_MODEL = 1024
BATCH = 4
SEQ = 8192
DEPTH = 1

D_MIX = D_MODEL
EPS = 1e-6
ROPE_THETA = 10000.0

MLA_HEADS = 8
MLA_QK_NOPE = 64
MLA_QK_ROPE = 32
MLA_QK_DIM = MLA_QK_NOPE + MLA_QK_ROPE
MLA_V_DIM = 64
MLA_Q_RANK = 256
MLA_KV_RANK = 128
MLA_WIDTH = MLA_HEADS * MLA_V_DIM
ATTN_BLOCK = 128

RET_HEADS = 8
RET_HEAD_DIM = 64
RET_WIDTH = RET_HEADS * RET_HEAD_DIM
RET_CHUNK = 128

IN_SPLITS = (
    MLA_Q_RANK,
    MLA_KV_RANK,
    MLA_QK_ROPE,
    RET_WIDTH, RET_WIDTH, RET_WIDTH, RET_WIDTH,
)
IN_COLS = sum(IN_SPLITS)

PEER_HEADS = 8
PEER_N_KEYS = 128
PEER_N_EXPERTS = PEER_N_KEYS * PEER_N_KEYS
PEER_KEY_DIM = 256
PEER_HALF = PEER_KEY_DIM // 2
PEER_TOPK = 16
PEER_TOKEN_BLOCK = 128

kernel_name = "hymba_mla_retnet_peer_adaln"


def rms_norm(x, g):
    xf = x.astype(jnp.float32)
    y = xf * lax.rsqrt(jnp.mean(xf * xf, axis=-1, keepdims=True) + EPS)
    return (y * g.astype(jnp.float32)).astype(x.dtype)


def rope(x, positions):
    d = x.shape[-1]
    inv = ROPE_THETA ** (-jnp.arange(0, d, 2, dtype=jnp.float32) / d)
    ang = positions.astype(jnp.float32)[:, :, None, None] * inv
    cos, sin = jnp.cos(ang), jnp.sin(ang)
    x1, x2 = jnp.split(x.astype(jnp.float32), 2, axis=-1)
    out = jnp.concatenate([x1 * cos - x2 * sin, x1 * sin + x2 * cos], axis=-1)
    return out.astype(x.dtype)


def mla_group(q_lat, kv_lat, k_rope, positions, g_q_norm, w_uq, g_kv_norm, w_ukv):
    B, S, _ = q_lat.shape
    q = (rms_norm(q_lat, g_q_norm) @ w_uq).reshape(B, S, MLA_HEADS, MLA_QK_DIM)
    q_nope, q_rope = q[..., :MLA_QK_NOPE], q[..., MLA_QK_NOPE:]
    q_rope = rope(q_rope, positions)
    kv = (rms_norm(kv_lat, g_kv_norm) @ w_ukv).reshape(B, S, MLA_HEADS, MLA_QK_NOPE + MLA_V_DIM)
    k_nope, v = kv[..., :MLA_QK_NOPE], kv[..., MLA_QK_NOPE:]
    k_r = rope(k_rope[:, :, None, :], positions)
    k = jnp.concatenate([k_nope, jnp.broadcast_to(k_r, (B, S, MLA_HEADS, MLA_QK_ROPE))], axis=-1)
    qh = jnp.concatenate([q_nope, q_rope], axis=-1) * (MLA_QK_DIM ** -0.5)

    nb = S // ATTN_BLOCK
    q_blocks = qh.reshape(B, nb, ATTN_BLOCK, MLA_HEADS, MLA_QK_DIM).transpose(1, 0, 2, 3, 4)
    key_pos = jnp.arange(S)

    def one_block(args):
        qb, bi = args
        s = jnp.einsum('bqhd,bkhd->bhqk', qb, k).astype(jnp.float32)
        q_pos = bi * ATTN_BLOCK + jnp.arange(ATTN_BLOCK)
        mask = key_pos[None, :] <= q_pos[:, None]
        s = jnp.where(mask[None, None], s, -jnp.inf)
        p = jax.nn.softmax(s, axis=-1).astype(v.dtype)
        return jnp.einsum('bhqk,bkhd->bqhd', p, v)

    out = lax.map(one_block, (q_blocks, jnp.arange(nb)))
    return out.transpose(1, 0, 2, 3, 4).reshape(B, S, MLA_WIDTH)


def retention_group(q, k, v, gate, positions, g_ret_norm):
    B, S, _ = q.shape
    H, d, C = RET_HEADS, RET_HEAD_DIM, RET_CHUNK
    nc = S // C
    q = rope(q.reshape(B, S, H, d), positions)
    k = rope(k.reshape(B, S, H, d), positions) * (d ** -0.5)
    v = v.reshape(B, S, H, d)

    gamma = 1.0 - 2.0 ** (-5.0 - jnp.arange(H, dtype=jnp.float32))
    log_g = jnp.log(gamma)
    idx = jnp.arange(C, dtype=jnp.float32)
    diff = idx[:, None] - idx[None, :]
    dmask = jnp.where(diff >= 0, jnp.exp(log_g[:, None, None] * jnp.maximum(diff, 0.0)), 0.0)

    qc = q.reshape(B, nc, C, H, d).astype(jnp.float32)
    kc = k.reshape(B, nc, C, H, d).astype(jnp.float32)
    vc = v.reshape(B, nc, C, H, d).astype(jnp.float32)

    scores = jnp.einsum('bnihd,bnjhd->bnhij', qc, kc) * dmask
    y_inner = jnp.einsum('bnhij,bnjhd->bnihd', scores, vc)

    zeta = jnp.exp(log_g[:, None] * (C - 1.0 - idx))
    kv_chunk = jnp.einsum('bnjhk,hj,bnjhv->bnhkv', kc, zeta, vc)
    chunk_decay = jnp.exp(log_g * C)

    def step(state, kv_n):
        return state * chunk_decay[None, :, None, None] + kv_n, state

    _, prev = lax.scan(step, jnp.zeros((B, H, d, d), jnp.float32), kv_chunk.transpose(1, 0, 2, 3, 4))
    prev = prev.transpose(1, 0, 2, 3, 4)
    xi = jnp.exp(log_g[:, None] * (idx + 1.0))
    y_cross = jnp.einsum('bnihk,bnhkv,hi->bnihv', qc, prev, xi)

    y = (y_inner + y_cross).reshape(B, S, H, d)
    mu = jnp.mean(y, axis=-1, keepdims=True)
    var = jnp.mean(jnp.square(y - mu), axis=-1, keepdims=True)
    yn = ((y - mu) * lax.rsqrt(var + EPS)).reshape(B, S, RET_WIDTH) * g_ret_norm.astype(jnp.float32)
    return (jax.nn.silu(gate.astype(jnp.float32)) * yn).astype(gate.dtype)


def peer(h, w_query, sub_keys, expert_u, expert_v):
    B, S, D = h.shape
    T = B * S
    K = PEER_TOPK
    xt = h.reshape(T, D)
    q = (xt @ w_query).reshape(T, PEER_HEADS, 2, PEER_HALF)
    s = jnp.einsum('thpd,pkd->thpk', q, sub_keys).astype(jnp.float32)
    s_top, i_top = lax.top_k(s, K)
    cand = (s_top[:, :, 0, :, None] + s_top[:, :, 1, None, :]).reshape(T, PEER_HEADS, K * K)
    cand_idx = (i_top[:, :, 0, :, None] * PEER_N_KEYS + i_top[:, :, 1, None, :]).reshape(T, PEER_HEADS, K * K)
    best, pos = lax.top_k(cand, K)
    eidx = jnp.take_along_axis(cand_idx, pos, axis=-1)
    g = jax.nn.softmax(best, axis=-1)

    nb = T // PEER_TOKEN_BLOCK

    def one_block(args):
        xb, ib, gb = args
        u = expert_u[ib]
        a = jax.nn.gelu(jnp.einsum('thkd,td->thk', u, xb).astype(jnp.float32), approximate=False)
        w = (gb * a).astype(expert_v.dtype)
        return jnp.einsum('thk,thkd->td', w, expert_v[ib])

    out = lax.map(one_block, (xt.reshape(nb, PEER_TOKEN_BLOCK, D),
                              eidx.reshape(nb, PEER_TOKEN_BLOCK, PEER_HEADS, K),
                              g.reshape(nb, PEER_TOKEN_BLOCK, PEER_HEADS, K)))
    return out.reshape(B, S, D)


def setup_inputs(seed: int = 0) -> dict:
    key = jax.random.key(seed)
    ks = jax.random.split(key, 20)
    L, D = DEPTH, D_MODEL
    nrm = lambda k, shape, scale: jax.random.normal(k, shape, jnp.float32) * scale
    gain = lambda k, shape: 1.0 + 0.02 * jax.random.normal(k, shape, jnp.float32)
    return {
        "x": nrm(ks[0], (BATCH, SEQ, D), 1.0),
        "c": nrm(ks[1], (BATCH, D), 1.0),
        "positions": jnp.broadcast_to(jnp.arange(SEQ, dtype=jnp.int32), (BATCH, SEQ)),
        "w_ada": nrm(ks[2], (L, D, 6 * D), D ** -0.5),
        "b_ada": nrm(ks[3], (L, 6 * D), 0.01),
        "g_norm1": gain(ks[4], (L, D)),
        "w_in": nrm(ks[5], (L, D, IN_COLS), D ** -0.5),
        "g_q_norm": gain(ks[6], (L, MLA_Q_RANK)),
        "w_uq": nrm(ks[7], (L, MLA_Q_RANK, MLA_HEADS * MLA_QK_DIM), MLA_Q_RANK ** -0.5),
        "g_kv_norm": gain(ks[8], (L, MLA_KV_RANK)),
        "w_ukv": nrm(ks[9], (L, MLA_KV_RANK, MLA_HEADS * (MLA_QK_NOPE + MLA_V_DIM)), MLA_KV_RANK ** -0.5),
        "g_ret_norm": gain(ks[10], (L, RET_WIDTH)),
        "w_out": nrm(ks[11], (L, MLA_WIDTH + RET_WIDTH, D), (MLA_WIDTH + RET_WIDTH) ** -0.5),
        "g_norm2": gain(ks[12], (L, D)),
        "w_query": nrm(ks[13], (L, D, PEER_HEADS * PEER_KEY_DIM), D ** -0.5),
        "sub_keys": nrm(ks[14], (L, 2, PEER_N_KEYS, PEER_HALF), PEER_HALF ** -0.5),
        "expert_u": nrm(ks[15], (L, PEER_N_EXPERTS, D), D ** -0.5),
        "expert_v": nrm(ks[16], (L, PEER_N_EXPERTS, D), 0.5),
        "g_final": gain(ks[17], (D,)),
    }


def reference(x, c, positions, w_ada, b_ada, g_norm1, w_in, g_q_norm, w_uq, g_kv_norm, w_ukv,
              g_ret_norm, w_out, g_norm2, w_query, sub_keys, expert_u, expert_v, g_final):
    offs = np.cumsum(IN_SPLITS)[:-1].tolist()
    for l in range(DEPTH):
        mod = jax.nn.silu(c) @ w_ada[l] + b_ada[l]
        sh1, sc1, gt1, sh2, sc2, gt2 = [m[:, None, :] for m in jnp.split(mod, 6, axis=-1)]

        h = rms_norm(x, g_norm1[l]) * (1.0 + sc1) + sh1
        proj = h @ w_in[l]
        q_lat, kv_lat, k_rope, rq, rk, rv, rg = jnp.split(proj, offs, axis=-1)
        y_mla = mla_group(q_lat, kv_lat, k_rope, positions, g_q_norm[l], w_uq[l], g_kv_norm[l], w_ukv[l])
        y_ret = retention_group(rq, rk, rv, rg, positions, g_ret_norm[l])
        mixed = jnp.concatenate([y_mla.astype(x.dtype), y_ret.astype(x.dtype)], axis=-1) @ w_out[l]
        x = x + gt1 * mixed

        h2 = rms_norm(x, g_norm2[l]) * (1.0 + sc2) + sh2
        x = x + gt2 * peer(h2, w_query[l], sub_keys[l], expert_u[l], expert_v[l]).astype(x.dtype)
    return rms_norm(x, g_final)
```

```python
import math
import os
from contextlib import ExitStack
import numpy as np
import concourse.bass as bass
import concourse.mybir as mybir
from concourse.bass_utils import run_bass_kernel_spmd

F32 = mybir.dt.float32
BF16 = mybir.dt.bfloat16
I32 = mybir.dt.int32
U32 = mybir.dt.uint32
ALU = mybir.AluOpType
AF = mybir.ActivationFunctionType
AX = mybir.AxisListType

EPS = 1e-6
NEG = -30000.0


class T:
    __slots__ = ("ap", "name", "lw", "rd")

    def __init__(self, ap, name=""):
        self.ap = ap
        self.name = name
        self.lw = None
        self.rd = []

    def __getitem__(self, k):
        return self.ap[k]


class FW:
    ENGS = ("pe", "act", "dve", "pool", "sp")

    def __init__(self, nc, n_dma_sems=16):
        self.nc = nc
        self.prog = {e: [] for e in self.ENGS}
        self.cnt = {e: 0 for e in self.ENGS}
        self.waited = {e: {} for e in self.ENGS}
        self.n_dma_sems = n_dma_sems
        self.dma_i = {"sp": 0, "pool": 0, "act": 0}
        self.dma_sem_val = {}
        self.sems = {}
        self.dma_last = {}

    def _need(self, eng, ev, waits):
        if ev is None:
            return
        k, v = ev
        if eng == "pe" and k == "pe":
            return
        if self.waited[eng].get(k, 0) >= v:
            return
        self.waited[eng][k] = v
        waits.append((k, v))

    def _deps(self, eng, reads, writes):
        waits = []
        for t in reads:
            self._need(eng, t.lw, waits)
        for t in writes:
            self._need(eng, t.lw, waits)
            for ev in t.rd:
                self._need(eng, ev, waits)
        m = {}
        for k, v in waits:
            m[k] = max(m.get(k, 0), v)
        return list(m.items())

    def _commit(self, ev, reads, writes):
        for t in reads:
            t.rd.append(ev)
            if len(t.rd) > 48:
                mm = {}
                for k, v in t.rd:
                    mm[k] = max(mm.get(k, 0), v)
                t.rd = list(mm.items())
        for t in writes:
            t.lw = ev
            t.rd = []

    def op(self, eng, fn, reads=(), writes=()):
        waits = self._deps(eng, reads, writes)
        self.cnt[eng] += 1
        ev = (eng, self.cnt[eng])
        self.prog[eng].append((waits, fn, (eng, 1)))
        self._commit(ev, reads, writes)
        return ev

    def dma(self, out, in_, reads=(), writes=(), q="sp", **kw):
        i = self.dma_i[q]
        self.dma_i[q] += 1
        k = "dma_%s_%d" % (q, i % self.n_dma_sems)
        waits = self._deps(q, reads, writes)
        prev = self.dma_last.get(k)
        if prev is not None:
            tmp = []
            self._need(q, prev, tmp)
            waits += tmp
        v = self.dma_sem_val.get(k, 0) + 16
        self.dma_sem_val[k] = v
        ev = (k, v)
        self.dma_last[k] = ev
        self.prog[q].append((waits, lambda e: e.dma_start(out=out, in_=in_, **kw), (k, 16)))
        self._commit(ev, reads, writes)
        return ev

    def barrier(self):
        evs = [(e, self.cnt[e]) for e in ("pe", "act", "dve", "pool") if self.cnt[e] > 0]
        evs += list(self.dma_last.values())
        for eng in self.ENGS:
            waits = []
            for ev in evs:
                if ev[0] == eng:
                    continue
                self._need(eng, ev, waits)
            if waits:
                self.prog[eng].append((waits, None, None))

    def final_wait(self, eng, tiles):
        waits = []
        for t in tiles:
            self._need(eng, t.lw, waits)
        if waits:
            self.prog[eng].append((waits, None, None))

    def emit(self, stack):
        nc = self.nc
        keys = list(self.ENGS)
        for q in ("sp", "pool", "act"):
            for j in range(min(self.n_dma_sems, self.dma_i[q])):
                keys.append("dma_%s_%d" % (q, j))
        for k in keys:
            self.sems[k] = stack.enter_context(nc.semaphore(k))
        block = stack.enter_context(nc.Block())
        sems = self.sems

        def run(eng_name):
            def body(e):
                for waits, fn, inc in self.prog[eng_name]:
                    for k, v in waits:
                        e.wait_ge(sems[k], v)
                    if fn is not None:
                        ins = fn(e)
                        ins.then_inc(sems[inc[0]], inc[1])
            return body

        block.tensor(run("pe"))
        block.scalar(run("act"))
        block.vector(run("dve"))
        block.gpsimd(run("pool"))
        block.sync(run("sp"))


TOK = 4096
NSLOT = 8192
GS = 512
NG = NSLOT // GS
PB = 256
NPB = TOK // PB
C_QL, C_KV, C_KR, C_KRS, C_RQ, C_RQS, C_RK, C_RKS, C_RV, C_RG = 0, 256, 384, 416, 448, 960, 1472, 1984, 2496, 3008
NCOL = 3520
ARENA_WORDS = 45 * 1024


def build(stop=None):
    nc = bass.Bass("TRN2", target_bir_lowering=False)
    st = ExitStack()
    fw = FW(nc)

    def din(name, shape, dt=F32):
        return nc.dram_tensor(name, list(shape), dt, kind="ExternalInput").ap()

    xo = din("xo", [TOK, 1024]); xp = din("xp", [TOK, 1024])
    pos = din("pos", [1, NSLOT], I32)
    cvec = din("cvec", [128, 8])
    w_ada = din("w_ada", [1024, 6144]); b_ada = din("b_ada", [1, 6144])
    g1 = din("g1", [1, 1024]); g2 = din("g2", [1, 1024]); gfin = din("gfin", [1, 1024])
    w_in = din("w_in", [1024, NCOL])
    w_uq = din("w_uq", [256, 1024]); w_ukv = din("w_ukv", [128, 1024])
    gq = din("gq", [128, 2]); gkv = din("gkv", [128, 1]); gret = din("gret", [1, 512])
    w_out = din("w_out", [1024, 1024]); w_query = din("w_query", [1024, 2048])
    skT = din("skT", [2, 128, 128])
    uT = din("uT", [128, 128, 1024]); vv = din("vv", [16384, 1024])
    ident_d = din("ident", [128, 128]); tri_d = din("tri", [128, 128]); iota_d = din("iota", [128, 128])
    rconst_d = din("rconst", [128, 12]); dq_d = din("dq", [128, 512]); dk_d = din("dk", [128, 512])
    decC_d = din("decC", [128, 4]); flag_d = din("flag", [128, 2])
    out_d = nc.dram_tensor("out", [TOK, 1024], F32, kind="ExternalOutput").ap()
    x1_d = nc.dram_tensor("x1s", [TOK, 1024], F32, kind="Internal").ap()
    ub_d = nc.dram_tensor("ubs", [128, 128, 1024], BF16, kind="Internal").ap()
    vb_d = nc.dram_tensor("vbs", [16384, 1024], BF16, kind="Internal").ap()
    DR = T(None, "dram_in")
    dbg_d = nc.dram_tensor("dbg", [128, 16384], F32, kind="ExternalOutput").ap() if stop else None
    DBG = T(dbg_d)
    dbg_pos = [0]
    dbg_stg = []

    def dump(t_, ap, cols):
        if not dbg_stg:
            dbg_stg.append(sbt("dstg", [128, 256]))
        stg = T(dbg_stg[0].ap[:, 0:cols], "stgv")
        stg = dbg_stg[0]
        rows = ap.shape[0]
        MEMSET("dve", stg.ap[:, 0:cols], 0.0, W=[stg])
        CP("dve", stg.ap[0:rows, 0:cols], ap, R=[t_], W=[stg])
        fw.dma(dbg_d[:, dbg_pos[0]:dbg_pos[0] + cols], stg.ap[:, 0:cols], reads=[stg], writes=[DBG])
        print("DBG", t_.name, dbg_pos[0], cols)
        dbg_pos[0] += cols

    def finish():
        fw.barrier()
        fw.emit(st)
        st.close()
        return nc

    X1 = T(x1_d); UB = T(ub_d); VB = T(vb_d); OUT = T(out_d)

    def sbt(name, shape, dt=F32):
        return T(st.enter_context(nc.sbuf_tensor("sb_" + name, list(shape), dt))[:], name)

    arena = st.enter_context(nc.sbuf_tensor("arena", [128, ARENA_WORDS], F32))
    apos = [0]

    def al(name, cols, dt=F32):
        words = (cols * (2 if dt == BF16 else 4) + 3) // 4
        a = arena[:, apos[0]:apos[0] + words]
        apos[0] += words
        assert apos[0] <= ARENA_WORDS, (name, apos[0])
        if dt != F32:
            a = a.bitcast(dt)
        return T(a, name)

    PS = [T(st.enter_context(nc.psum_tensor("ps%d" % i, [128, 512], F32))[:, :], "ps%d" % i) for i in range(8)]

    def MM(out, lhsT, rhs, start=True, stop=True, R=(), W=()):
        fw.op("pe", lambda e: e.matmul(out, lhsT, rhs, start=start, stop=stop), reads=R, writes=W)

    def TR(out, in_, idn, R=(), W=()):
        fw.op("pe", lambda e: e.transpose(out, in_, idn), reads=R, writes=W)

    def ACT(out, in_, func, R=(), W=(), **kw):
        fw.op("act", lambda e: e.activation(out, in_, func, **kw), reads=R, writes=W)

    def TTo(eng, out, in0, in1, op, R=(), W=()):
        fw.op(eng, lambda e: e.tensor_tensor(out, in0, in1, op), reads=R, writes=W)

    def TS(eng, out, in0, s1, s2, op0, op1=None, R=(), W=()):
        if op1 is None:
            fw.op(eng, lambda e: e.tensor_scalar(out, in0, s1, None, op0), reads=R, writes=W)
        else:
            fw.op(eng, lambda e: e.tensor_scalar(out, in0, s1, s2, op0, op1), reads=R, writes=W)

    def STT(eng, out, in0, sc, in1, op0, op1, R=(), W=()):
        fw.op(eng, lambda e: e.scalar_tensor_tensor(out, in0, sc, in1, op0, op1), reads=R, writes=W)

    def CP(eng, out, in_, R=(), W=()):
        fw.op(eng, lambda e: e.tensor_copy(out, in_), reads=R, writes=W)

    def RSUM(out, in_, R=(), W=()):
        fw.op("dve", lambda e: e.reduce_sum(out, in_, AX.X), reads=R, writes=W)

    def RECIP(out, in_, R=(), W=()):
        fw.op("dve", lambda e: e.reciprocal(out, in_), reads=R, writes=W)

    def MEMSET(eng, ap, val, W=()):
        fw.op(eng, lambda e: e.memset(ap, val), writes=W)

    def rstd(out_t, in_ap, scale, R=()):
        o = in_ap
        TS("dve", o, in_ap, scale, EPS, ALU.mult, ALU.add, R=list(R), W=[out_t])
        fw.op("act", lambda e: e.sqrt(o, o), reads=[out_t], writes=[out_t])
        RECIP(o, o, R=[out_t], W=[out_t])

    idf = sbt("idf", [128, 128]); idb = sbt("idb", [128, 128], BF16)
    trib = sbt("trib", [128, 128], BF16); trif = sbt("trif", [128, 128])
    iotab = sbt("iotab", [128, 128], BF16); iotaf = sbt("iotaf", [128, 128])
    rconst = sbt("rconst", [128, 12]); dq = sbt("dq", [128, 512]); dk = sbt("dk", [128, 512])
    decC = sbt("decC", [128, 4]); flag = sbt("flag", [128, 2])
    onesb = sbt("onesb", [128, 128], BF16)
    gt1b = sbt("gt1b", [128, 1024]); gt2b = sbt("gt2b", [128, 1024]); gfinb = sbt("gfinb", [128, 1024])
    gretb = sbt("gretb", [128, 512])
    a1c = sbt("a1c", [128, 8]); sh1c = sbt("sh1c", [128, 8]); a2c = sbt("a2c", [128, 8]); sh2c = sbt("sh2c", [128, 8])
    gqc = sbt("gqc", [128, 2]); gkvc = sbt("gkvc", [128, 1])
    wuq = sbt("wuq", [128, 2, 1024], BF16); wukv = sbt("wukv", [128, 1024], BF16)
    skb = sbt("skb", [128, 2, 128], BF16)

    for t_, d_ in ((idf, ident_d), (trif, tri_d), (iotaf, iota_d), (rconst, rconst_d), (dq, dq_d), (dk, dk_d),
                   (decC, decC_d), (flag, flag_d), (gqc, gq), (gkvc, gkv)):
        fw.dma(t_.ap[:], d_[:, :], reads=[DR], writes=[t_])
    CP("dve", idb.ap[:], idf.ap[:], R=[idf], W=[idb])
    CP("dve", trib.ap[:], trif.ap[:], R=[trif], W=[trib])
    CP("dve", iotab.ap[:], iotaf.ap[:], R=[iotaf], W=[iotab])
    MEMSET("dve", onesb.ap[:], 1.0, W=[onesb])
    fw.dma(gfinb.ap[:], gfin.partition_broadcast(128), reads=[DR], writes=[gfinb])
    fw.dma(gretb.ap[:], gret.partition_broadcast(128), reads=[DR], writes=[gretb])
    fw.dma(wuq.ap[:], w_uq.rearrange("(k p) n -> p k n", p=128), reads=[DR], writes=[wuq], q="pool")
    fw.dma(wukv.ap[:], w_ukv[:, :], reads=[DR], writes=[wukv], q="pool")
    fw.dma(skb.ap[:], skT.rearrange("a p n -> p a n"), reads=[DR], writes=[skb], q="pool")
    for i in (range(0, 128, 8) if 'nocast' not in os.environ.get('KSKIP', '') else []):
        fw.dma(ub_d[i:i + 8].rearrange("i p n -> p i n"), uT[i:i + 8].rearrange("i p n -> p i n"),
               reads=[DR], writes=[UB], q="pool")
        fw.dma(vb_d[i * 128:(i + 8) * 128, :].rearrange("(i p) n -> p i n", p=128),
               vv[i * 128:(i + 8) * 128, :].rearrange("(i p) n -> p i n", p=128), reads=[DR], writes=[VB], q="pool")

    apos[0] = 0
    cv = al("cv", 8); scv = al("scv", 8); lrep = al("lrep", 8 * 128, BF16)
    modbc = al("modbc", 6144); wa = [al("wa0", 3072, BF16), al("wa1", 3072, BF16)]
    tmpa = al("tmpa", 1024); g1bc = al("g1bc", 1024); g2bc = al("g2bc", 1024)
    fw.dma(cv.ap[:], cvec[:, :], reads=[DR], writes=[cv])
    ACT(scv.ap[:], cv.ap[:], AF.Silu, R=[cv], W=[scv])
    CP("dve", lrep.ap.rearrange("p (k m) -> p k m", k=8), scv.ap.unsqueeze(2).to_broadcast([128, 8, 128]), R=[scv], W=[lrep])
    fw.dma(modbc.ap[:], b_ada.partition_broadcast(128), reads=[DR], writes=[modbc])
    fw.dma(g1bc.ap[:], g1.partition_broadcast(128), reads=[DR], writes=[g1bc])
    fw.dma(g2bc.ap[:], g2.partition_broadcast(128), reads=[DR], writes=[g2bc])
    di = 0
    for half in range(2):
        for kc in range(8):
            w = wa[di % 2]; di += 1
            fw.dma(w.ap[:], w_ada[kc * 128:(kc + 1) * 128, half * 3072:(half + 1) * 3072], reads=[DR], writes=[w], q="pool")
            for nt in range(6):
                MM(PS[nt].ap[:, :], lrep.ap[:, kc * 128:(kc + 1) * 128], w.ap[:, nt * 512:(nt + 1) * 512],
                   start=(kc == 0), stop=(kc == 7), R=[lrep, w], W=[PS[nt]])
        for nt in range(6):
            sl = modbc.ap[:, half * 3072 + nt * 512: half * 3072 + (nt + 1) * 512]
            TTo("dve", sl, PS[nt].ap[:, :], sl, ALU.add, R=[PS[nt], modbc], W=[modbc])
    CP("dve", gt1b.ap[:], modbc.ap[:, 2048:3072], R=[modbc], W=[gt1b])
    CP("dve", gt2b.ap[:], modbc.ap[:, 5120:6144], R=[modbc], W=[gt2b])
    idf3 = idf.ap.unsqueeze(1).to_broadcast([128, 8, 128])

    def diag_extract(dst, src_ap, R):
        t3 = tmpa.ap.rearrange("p (k m) -> p k m", k=8)
        TTo("dve", t3, src_ap.rearrange("p (k m) -> p k m", k=8), idf3, ALU.mult, R=list(R) + [idf], W=[tmpa])
        RSUM(dst.ap[:], t3, R=[tmpa], W=[dst])

    for (sc_off, sh_off, gbc, ac, shc) in ((1024, 0, g1bc, a1c, sh1c), (4096, 3072, g2bc, a2c, sh2c)):
        STT("dve", gbc.ap[:], modbc.ap[:, sc_off:sc_off + 1024], 1.0, gbc.ap[:], ALU.add, ALU.mult, R=[modbc, gbc], W=[gbc])
        diag_extract(ac, gbc.ap, [gbc])
        diag_extract(shc, modbc.ap[:, sh_off:sh_off + 1024], [modbc])
    fw.barrier()
    if stop == "S0":
        for t_ in (a1c, sh1c, a2c, sh2c):
            dump(t_, t_.ap[:, 0:8], 8)
        dump(gt1b, gt1b.ap[:, 0:64], 64); dump(gt2b, gt2b.ap[:, 0:64], 64)
        return finish()

    def slot_src(tile_idx):
        if tile_idx < 32:
            return xp[tile_idx * 128:(tile_idx + 1) * 128, :]
        return xo[(tile_idx - 32) * 128:(tile_idx - 31) * 128, :]

    def norm_transpose(xt, xn, ss, hT, col0, acol, shcol, psb):
        junk = xn
        ACT(xn.ap[:], xt.ap[:], AF.Square, R=[xt], W=[xn, ss], accum_out=ss.ap[:, 0:1])
        rstd(ss, ss.ap[:, 0:1], 1.0 / 1024.0, R=[ss])
        ACT(xn.ap[:], xt.ap[:], AF.Copy, R=[xt, ss], W=[xn], scale=ss.ap[:, 0:1])
        pb = psb.ap.bitcast(BF16)
        for kc in range(8):
            TR(pb[:, kc * 128:(kc + 1) * 128], xn.ap[:, kc * 128:(kc + 1) * 128], idb.ap[:], R=[xn, idb], W=[psb])
        for kc in range(8):
            eng = "dve" if kc % 2 == 0 else "pool"
            TS("dve", hT.ap[:, kc * GSW + col0: kc * GSW + col0 + 128], pb[:, kc * 128:(kc + 1) * 128],
               acol.ap[:, kc:kc + 1], shcol.ap[:, kc:kc + 1], ALU.mult, ALU.add, R=[psb, acol, shcol], W=[hT])

    def rope_table(out_t, posf, inv_c, ph_c, sg_c, rows, tmp, tmpi, kf):
        n = GS
        a = tmp.ap[0:rows, 0:n]; ki = tmpi.ap[0:rows, 0:n]; k = kf.ap[0:rows, 0:n]
        TS("dve", a, posf.ap[0:rows, 0:n], rconst.ap[0:rows, inv_c:inv_c + 1], rconst.ap[0:rows, ph_c:ph_c + 1],
           ALU.mult, ALU.add, R=[posf, rconst], W=[tmp])
        TS("dve", k, a, 1.0 / (2.0 * math.pi), None, ALU.mult, R=[tmp], W=[kf])
        CP("dve", ki, k, R=[kf], W=[tmpi])
        CP("dve", k, ki, R=[tmpi], W=[kf])
        STT("dve", a, k, -2.0 * math.pi, a, ALU.mult, ALU.add, R=[kf, tmp], W=[tmp])
        TS("dve", a, a, 3.1415925, -3.1415925, ALU.min, ALU.max, R=[tmp], W=[tmp])
        ACT(a, a, AF.Sin, R=[tmp], W=[tmp])
        TS("dve", out_t.ap[0:rows, 0:n], a, rconst.ap[0:rows, sg_c:sg_c + 1], None, ALU.mult, R=[tmp, rconst], W=[out_t])

    apos[0] = 0
    ymT = al("ymT", 4 * TOK, BF16)
    mark_Y = apos[0]
    qnT = al("qnT", 2 * TOK, BF16)
    kvnT = al("kvnT", NSLOT, BF16)
    KT = al("KT", NSLOT, BF16)
    CQ = al("CQ", TOK, BF16); SQ = al("SQ", TOK, BF16)
    mark_A1 = apos[0]
    GSW = GS
    wA = al("wA", 8 * 448, BF16)
    xts = [al("xtA%d" % i, 1024) for i in range(2)]
    xn = al("xn", 1024, BF16); ss = al("ss", 4)
    hT = al("hT", 8 * GS, BF16)
    posi = al("posi", GS, I32); posf = al("posf", GS)
    tmpt = al("tmpt", GS); mC = al("mC", GS); mS = al("mS", GS); tmpi = al("tmpi", GS, I32); kfl = al("kfl", GS)
    sqb = al("sqb", 2 * GS, BF16); rbc = al("rbc", GS); t1 = al("t1", GS); t2 = al("t2", GS)
    fw.dma(wA.ap.rearrange("p (k n) -> p k n", k=8), w_in[:, 0:448].rearrange("(k p) n -> p k n", p=128),
           reads=[DR], writes=[wA], q="pool")

    def load_pos(g):
        fw.dma(posi.ap[:], pos[:, g * GS:(g + 1) * GS].partition_broadcast(128), reads=[DR], writes=[posi])
        CP("dve", posf.ap[:], posi.ap[:], R=[posi], W=[posf])

    def lat_norm(ps_list, gcol, ncol_div, extra_scale, dst_aps, dstT):
        n = len(ps_list)
        for i, p in enumerate(ps_list):
            ACT(sqb.ap[:, i * GS:(i + 1) * GS], p.ap[:, :], AF.Square, R=[p], W=[sqb])
        for i in range(n):
            MM(PS[7].ap[:, :], onesb.ap[:], sqb.ap[:, i * GS:(i + 1) * GS], start=(i == 0), stop=(i == n - 1), R=[onesb, sqb], W=[PS[7]])
        TS("dve", rbc.ap[:], PS[7].ap[:, :], 1.0 / ncol_div, EPS, ALU.mult, ALU.add, R=[PS[7]], W=[rbc])
        fw.op("act", lambda e: e.sqrt(rbc.ap[:], rbc.ap[:]), reads=[rbc], writes=[rbc])
        RECIP(rbc.ap[:], rbc.ap[:], R=[rbc], W=[rbc])
        if extra_scale != 1.0:
            TS("dve", rbc.ap[:], rbc.ap[:], extra_scale, None, ALU.mult, R=[rbc], W=[rbc])
        for i, p in enumerate(ps_list):
            STT("dve", dst_aps[i], p.ap[:, :], gcol.ap[:, i:i + 1], rbc.ap[:], ALU.mult, ALU.mult, R=[p, gcol, rbc], W=[dstT])

    for g in range(NG):
        own = g >= 8
        for tt in range(4):
            xt = xts[tt % 2]
            fw.dma(xt.ap[:], slot_src(g * 4 + tt), reads=[DR], writes=[xt])
            norm_transpose(xt, xn, ss, hT, tt * 128, a1c, sh1c, PS[6])
        load_pos(g)
        rope_table(mC, posf, 0, 1, 2, 32, tmpt, tmpi, kfl)
        rope_table(mS, posf, 0, 3, 4, 32, tmpt, tmpi, kfl)
        wA3 = wA.ap.rearrange("p (k n) -> p k n", k=8)
        hT3 = hT.ap.rearrange("p (k n) -> p k n", k=8)

        def proj(ps, c0, m):
            for kc in range(8):
                MM(ps.ap[0:m, :], wA3[:, kc, c0:c0 + m], hT3[:, kc, :], start=(kc == 0), stop=(kc == 7), R=[wA, hT], W=[ps])
        proj(PS[2], C_KV, 128)
        lat_norm([PS[2]], gkvc, 128.0, 1.0, [kvnT.ap[:, g * GS:(g + 1) * GS]], kvnT)
        proj(PS[3], C_KR, 32); proj(PS[4], C_KRS, 32)
        TTo("dve", t1.ap[0:32, :], PS[3].ap[0:32, :], mC.ap[0:32, :], ALU.mult, R=[PS[3], mC], W=[t1])
        TTo("dve", t2.ap[0:32, :], PS[4].ap[0:32, :], mS.ap[0:32, :], ALU.mult, R=[PS[4], mS], W=[t2])
        TTo("dve", t1.ap[0:32, :], t1.ap[0:32, :], t2.ap[0:32, :], ALU.add, R=[t1, t2], W=[t1])
        ACT(KT.ap[64:96, g * GS:(g + 1) * GS], t1.ap[0:32, :], AF.Copy, R=[t1], W=[KT])
        if own:
            og = g - 8
            proj(PS[0], C_QL, 128); proj(PS[1], C_QL + 128, 128)
            qn3 = qnT.ap.rearrange("p (k n) -> p k n", k=2)
            lat_norm([PS[0], PS[1]], gqc, 256.0, 96.0 ** -0.5,
                     [qn3[:, 0, og * GS:(og + 1) * GS], qn3[:, 1, og * GS:(og + 1) * GS]], qnT)
            CP("dve", CQ.ap[0:32, og * GS:(og + 1) * GS], mC.ap[0:32, :], R=[mC], W=[CQ])
            CP("dve", SQ.ap[0:32, og * GS:(og + 1) * GS], mS.ap[0:32, :], R=[mS], W=[SQ])
    fw.barrier()
    if stop == "A1":
        dump(kvnT, kvnT.ap[:, 0:256], 256); dump(kvnT, kvnT.ap[:, 4096:4352], 256)
        dump(qnT, qnT.ap[:, 0:256], 256); dump(qnT, qnT.ap[:, 4096:4352], 256)
        dump(KT, KT.ap[64:96, 0:256], 256); dump(KT, KT.ap[64:96, 4096 + 3840:8192], 256)
        return finish()

    apos[0] = mark_A1
    t1 = al("t1B", GS); t2 = al("t2B", GS)
    Vaug = [al("Vaug0", 64 * 128, BF16), al("Vaug1", 64 * 128, BF16)]
    QT = al("QT", TOK, BF16)
    PT = [al("PT%d" % i, 512, BF16) for i in range(4)]
    lsb = al("lsb", 512)
    kb_col = flag.ap[:, 1:2]
    MEMSET("dve", Vaug[0].ap[:], 1.0, W=[Vaug[0]])
    MEMSET("pool", Vaug[1].ap[:], 1.0, W=[Vaug[1]])
    ym3 = ymT.ap.rearrange("p (k n) -> p k n", k=4)
    pti = 0
    for h in range(8):
        par = h % 2
        Va = Vaug[par]
        Va3 = Va.ap.rearrange("p (b n) -> p b n", b=64)
        voff = 0 if par == 0 else 64
        for g in range(NG):
            ps = PS[g % 2]
            MM(ps.ap[0:64, :], wukv.ap[:, h * 64:(h + 1) * 64], kvnT.ap[:, g * GS:(g + 1) * GS], R=[wukv, kvnT], W=[ps])
            if g % 2 == 0:
                CP("dve", KT.ap[0:64, g * GS:(g + 1) * GS], ps.ap[0:64, :], R=[ps], W=[KT])
            else:
                ACT(KT.ap[0:64, g * GS:(g + 1) * GS], ps.ap[0:64, :], AF.Copy, R=[ps], W=[KT])
        for b8 in range(8):
            ps = PS[2 + b8 % 2]
            for j in range(8):
                kb = b8 * 8 + j
                MM(ps.ap[:, j * 64:(j + 1) * 64], kvnT.ap[:, kb * 128:(kb + 1) * 128], wukv.ap[:, 512 + h * 64: 512 + (h + 1) * 64],
                   R=[wukv, kvnT], W=[ps])
            CP("dve", Va3[:, b8 * 8:(b8 + 1) * 8, voff:voff + 64], ps.ap.rearrange("p (b n) -> p b n", b=8), R=[ps], W=[Va])
        wq3 = wuq.ap
        for og in range(8):
            sl = slice(og * GS, (og + 1) * GS)
            qn3 = qnT.ap.rearrange("p (k n) -> p k n", k=2)
            for (ps, c0, m) in ((PS[4], h * 64, 64), (PS[5], 512 + h * 32, 32), (PS[6], 768 + h * 32, 32)):
                for kt in range(2):
                    MM(ps.ap[0:m, :], wq3[:, kt, c0:c0 + m], qn3[:, kt, sl], start=(kt == 0), stop=(kt == 1), R=[wuq, qnT], W=[ps])
            ACT(QT.ap[0:64, sl], PS[4].ap[0:64, :], AF.Copy, R=[PS[4]], W=[QT])
            TTo("dve", t1.ap[0:32, :], PS[5].ap[0:32, :], CQ.ap[0:32, sl], ALU.mult, R=[PS[5], CQ], W=[t1])
            TTo("dve", t2.ap[0:32, :], PS[6].ap[0:32, :], SQ.ap[0:32, sl], ALU.mult, R=[PS[6], SQ], W=[t2])
            TTo("dve", t1.ap[0:32, :], t1.ap[0:32, :], t2.ap[0:32, :], ALU.add, R=[t1, t2], W=[t1])
            ACT(QT.ap[64:96, sl], t1.ap[0:32, :], AF.Copy, R=[t1], W=[QT])
        for og in range(8):
            qsl0 = og * GS
            nkb = 32 + 4 * (og + 1)
            po = PS[6 + og % 2]

            def att_front(kb):
                ps = PS[kb % 2]
                j = kb - 32 - 4 * og
                c0 = 128 * j if j > 0 else 0
                MM(ps.ap[:, c0:512], KT.ap[0:96, kb * 128:(kb + 1) * 128], QT.ap[0:96, qsl0 + c0: qsl0 + 512], R=[KT, QT], W=[ps])
                pt = PT[kb % 4]
                if kb < 32:
                    ACT(pt.ap[:, c0:512], ps.ap[:, c0:512], AF.Exp, R=[ps, flag], W=[pt], bias=kb_col)
                else:
                    ACT(pt.ap[:, c0:512], ps.ap[:, c0:512], AF.Exp, R=[ps], W=[pt])
                if j >= 0:
                    TTo("pool", pt.ap[:, c0:c0 + 128], pt.ap[:, c0:c0 + 128], trib.ap[:], ALU.mult, R=[pt, trib], W=[pt])
                return (kb, pt, c0)

            def att_back(item):
                kb, pt, c0 = item
                MM(po.ap[:, c0:512], Va3[:, kb, :], pt.ap[:, c0:512], start=(kb == 0), stop=(kb == nkb - 1), R=[Va, pt], W=[po])

            pend = None
            for kb in range(nkb):
                cur = att_front(kb)
                if pend is not None:
                    att_back(pend)
                pend = cur
            att_back(pend)
            osl = slice(qsl0, qsl0 + 512)
            if par == 0:
                ACT(lsb.ap[0:64, :], po.ap[64:128, :], AF.Copy, R=[po], W=[lsb])
                RECIP(lsb.ap[0:64, :], lsb.ap[0:64, :], R=[lsb], W=[lsb])
                TTo("dve", ym3[0:64, h // 2, osl], po.ap[0:64, :], lsb.ap[0:64, :], ALU.mult, R=[po, lsb], W=[ymT])
            else:
                ACT(lsb.ap[64:128, :], po.ap[0:64, :], AF.Copy, R=[po], W=[lsb])
                RECIP(lsb.ap[64:128, :], lsb.ap[64:128, :], R=[lsb], W=[lsb])
                TTo("dve", ym3[64:128, h // 2, osl], po.ap[64:128, :], lsb.ap[64:128, :], ALU.mult, R=[po, lsb], W=[ymT])
    fw.barrier()
    if stop == "B":
        for pr_ in range(4):
            dump(ymT, ym3[:, pr_, 0:256], 256); dump(ymT, ym3[:, pr_, 3840:4096], 256)
        return finish()

    apos[0] = mark_Y
    NR = 3072
    wR = al("wR", 8 * NR, BF16)
    wO = al("wO", 8 * 1024, BF16)
    xts = [al("xtR%d" % i, 1024) for i in range(4)]
    xn = al("xn2", 1024, BF16); ss = al("ss2", 4)
    hT = al("hT2", 8 * GS, BF16)
    posi = al("posi2", GS, I32); posf = al("posf2", GS)
    tmpt = al("tmpt2", GS); rC = al("rC", GS); rS = al("rS", GS); tmpi = al("tmpi2", GS, I32); kfl = al("kfl2", GS)
    t1 = al("t1b", GS); t2 = al("t2b", GS)
    QrT = al("QrT", 4 * GS, BF16); KrT = al("KrT", 4 * GS, BF16)
    Ktok = al("Ktok", 4 * 512, BF16); Vtok = al("Vtok", 4 * 512, BF16); gate = al("gate", 4 * 512, BF16)
    Sm = al("Sm", 8 * 128, BF16); ysb = al("ysb", 512); ysq = al("ysq", 512); yg = al("yg", 512, BF16)
    yrT = al("yrT", 4 * GS, BF16)
    stT = al("stT", 4 * 64); stbF = al("stbF", 4 * 128, BF16)
    st8 = al("st8", 64)
    fw.dma(wR.ap.rearrange("p (k n) -> p k n", k=8), w_in[:, 448:NCOL].rearrange("(k p) n -> p k n", p=128),
           reads=[DR], writes=[wR], q="pool")
    fw.dma(wO.ap.rearrange("p (k n) -> p k n", k=8), w_out.rearrange("(k p) n -> p k n", p=128), reads=[DR], writes=[wO], q="pool")
    MEMSET("dve", stT.ap[:], 0.0, W=[stT])
    MEMSET("dve", stbF.ap[:], 0.0, W=[stbF])
    wR3 = wR.ap.rearrange("p (k n) -> p k n", k=8)
    wO3 = wO.ap.rearrange("p (k n) -> p k n", k=8)
    hT3 = hT.ap.rearrange("p (k n) -> p k n", k=8)
    stT3 = stT.ap.rearrange("p (a n) -> p a n", a=4)
    sbF3 = stbF.ap.rearrange("p (a n) -> p a n", a=4)
    dq3 = dq.ap.rearrange("p (a n) -> p a n", a=4)
    dk3 = dk.ap.rearrange("p (a n) -> p a n", a=4)
    Qr3 = QrT.ap.rearrange("p (a n) -> p a n", a=4)
    Kr3 = KrT.ap.rearrange("p (a n) -> p a n", a=4)
    Kt3 = Ktok.ap.rearrange("p (c n) -> p c n", c=4)
    Vt3 = Vtok.ap.rearrange("p (c n) -> p c n", c=4)
    gt3 = gate.ap.rearrange("p (c n) -> p c n", c=4)
    yr3 = yrT.ap.rearrange("p (a n) -> p a n", a=4)

    def rope_ret(dst3, c_plain, c_sw, dec3):
        for p in range(4):
            for (ps, c0) in ((PS[0], c_plain + p * 128), (PS[1], c_sw + p * 128)):
                for kc in range(8):
                    MM(ps.ap[:, :], wR3[:, kc, c0 - 448:c0 - 448 + 128], hT3[:, kc, :], start=(kc == 0), stop=(kc == 7), R=[wR, hT], W=[ps])
            TTo("dve", t1.ap[:], PS[0].ap[:, :], rC.ap[:], ALU.mult, R=[PS[0], rC], W=[t1])
            TTo("dve", t2.ap[:], PS[1].ap[:, :], rS.ap[:], ALU.mult, R=[PS[1], rS], W=[t2])
            TTo("dve", t1.ap[:], t1.ap[:], t2.ap[:], ALU.add, R=[t1, t2], W=[t1])
            TTo("dve", dst3[:, p, :].rearrange("p (c i) -> p c i", c=4), t1.ap.rearrange("p (c i) -> p c i", c=4),
                dec3[:, p, :].unsqueeze(1).to_broadcast([128, 4, 128]), ALU.mult, R=[t1], W=[QrT if dst3 is Qr3 else KrT])

    for g in range(NG):
        own = g >= 8
        og = g - 8
        for tt in range(4):
            xt = xts[tt]
            fw.dma(xt.ap[:], slot_src(g * 4 + tt), reads=[DR], writes=[xt])
            norm_transpose(xt, xn, ss, hT, tt * 128, a1c, sh1c, PS[6])
        fw.dma(posi.ap[:], pos[:, g * GS:(g + 1) * GS].partition_broadcast(128), reads=[DR], writes=[posi])
        CP("dve", posf.ap[:], posi.ap[:], R=[posi], W=[posf])
        rope_table(rC, posf, 5, 6, 7, 128, tmpt, tmpi, kfl)
        rope_table(rS, posf, 5, 8, 9, 128, tmpt, tmpi, kfl)
        rope_ret(Kr3, C_RK, C_RKS, dk3)
        if own:
            rope_ret(Qr3, C_RQ, C_RQS, dq3)
        for c in range(4):
            pb = PS[2].ap.bitcast(BF16)
            for p in range(4):
                TR(pb[:, p * 128:(p + 1) * 128], Kr3[:, p, c * 128:(c + 1) * 128], idb.ap[:], R=[KrT, idb], W=[PS[2]])
            CP("dve", Kt3[:, c, :], pb[:, 0:512], R=[PS[2]], W=[Ktok])
            for kc in range(8):
                MM(PS[3].ap[:, :], hT3[:, kc, c * 128:(c + 1) * 128], wR3[:, kc, C_RV - 448:C_RV - 448 + 512],
                   start=(kc == 0), stop=(kc == 7), R=[wR, hT], W=[PS[3]])
            ACT(Vt3[:, c, :], PS[3].ap[:, :], AF.Copy, R=[PS[3]], W=[Vtok])
            if own:
                for kc in range(8):
                    MM(PS[4].ap[:, :], hT3[:, kc, c * 128:(c + 1) * 128], wR3[:, kc, C_RG - 448:C_RG - 448 + 512],
                       start=(kc == 0), stop=(kc == 7), R=[wR, hT], W=[PS[4]])
                ACT(gt3[:, c, :], PS[4].ap[:, :], AF.Silu, R=[PS[4]], W=[gate])
        if g == 8:
            TS("dve", stT.ap[:], stT.ap[:], flag.ap[:, 0:1], None, ALU.mult, R=[stT, flag], W=[stT])
        for c in range(4):
            csl = slice(c * 128, (c + 1) * 128)
            if own:
                for r0 in (0, 64):
                    TTo("dve", sbF3[r0:r0 + 64, :, r0:r0 + 64], stT3[r0:r0 + 64, :, :],
                        decC.ap[r0:r0 + 64, :].unsqueeze(2).to_broadcast([64, 4, 64]), ALU.mult, R=[stT, decC], W=[stbF])
                for h in range(8):
                    p, r0 = h // 2, (h % 2) * 64
                    ps = PS[h % 2]
                    MM(ps.ap[:, p * 128:(p + 1) * 128], Kr3[r0:r0 + 64, p, csl], Qr3[r0:r0 + 64, p, csl], R=[KrT, QrT], W=[ps])
                tri3 = trif.ap.unsqueeze(1).to_broadcast([128, 4, 128])
                for hh in range(2):
                    TTo("dve", Sm.ap[:, hh * 512:(hh + 1) * 512].rearrange("p (a n) -> p a n", a=4),
                        PS[hh].ap.rearrange("p (a n) -> p a n", a=4), tri3, ALU.mult, R=[PS[hh], trif], W=[Sm])
                for p in range(4):
                    MM(PS[5].ap[:, p * 128:(p + 1) * 128], Qr3[:, p, csl], sbF3[:, p, :], start=True, stop=False, R=[QrT, stbF], W=[PS[5]])
                    for s2 in range(2):
                        h = 2 * p + s2
                        smb = s2 * 4 + p
                        MM(PS[5].ap[:, h * 64:(h + 1) * 64], Sm.ap[:, smb * 128:(smb + 1) * 128], Vt3[:, c, h * 64:(h + 1) * 64],
                           start=False, stop=True, R=[Sm, Vtok], W=[PS[5]])
                ACT(ysb.ap[:], PS[5].ap[:, :], AF.Copy, R=[PS[5]], W=[ysb])
                ACT(ysq.ap[:], PS[5].ap[:, :], AF.Square, R=[PS[5]], W=[ysq])
                y3 = ysb.ap.rearrange("p (h n) -> p h n", h=8)
                q3 = ysq.ap.rearrange("p (h n) -> p h n", h=8)
                RSUM(st8.ap[:, 0:8], y3, R=[ysb], W=[st8])
                RSUM(st8.ap[:, 8:16], q3, R=[ysq], W=[st8])
                TS("dve", st8.ap[:, 0:8], st8.ap[:, 0:8], 1.0 / 64.0, None, ALU.mult, R=[st8], W=[st8])
                TTo("dve", st8.ap[:, 16:24], st8.ap[:, 0:8], st8.ap[:, 0:8], ALU.mult, R=[st8], W=[st8])
                STT("dve", st8.ap[:, 8:16], st8.ap[:, 8:16], 1.0 / 64.0, st8.ap[:, 16:24], ALU.mult, ALU.subtract, R=[st8], W=[st8])
                TS("dve", st8.ap[:, 8:16], st8.ap[:, 8:16], EPS, None, ALU.add, R=[st8], W=[st8])
                fw.op("act", lambda e: e.sqrt(st8.ap[:, 8:16], st8.ap[:, 8:16]), reads=[st8], writes=[st8])
                RECIP(st8.ap[:, 8:16], st8.ap[:, 8:16], R=[st8], W=[st8])
                TTo("dve", y3, y3, st8.ap[:, 0:8].unsqueeze(2).to_broadcast([128, 8, 64]), ALU.subtract, R=[ysb, st8], W=[ysb])
                TTo("dve", y3, y3, st8.ap[:, 8:16].unsqueeze(2).to_broadcast([128, 8, 64]), ALU.mult, R=[ysb, st8], W=[ysb])
                TTo("dve", ysb.ap[:], ysb.ap[:], gretb.ap[:], ALU.mult, R=[ysb, gretb], W=[ysb])
                TTo("dve", yg.ap[:], ysb.ap[:], gt3[:, c, :], ALU.mult, R=[ysb, gate], W=[yg])
                pb = PS[2].ap.bitcast(BF16)
                for p in range(4):
                    TR(pb[:, p * 128:(p + 1) * 128], yg.ap[:, p * 128:(p + 1) * 128], idb.ap[:], R=[yg, idb], W=[PS[2]])
                CP("dve", yr3[:, :, csl], pb[:, 0:512].rearrange("p (a n) -> p a n", a=4), R=[PS[2]], W=[yrT])
            for p in range(4):
                MM(PS[3].ap[:, p * 128:(p + 1) * 128], Kt3[:, c, p * 128:(p + 1) * 128], Vt3[:, c, p * 128:(p + 1) * 128], R=[Ktok, Vtok], W=[PS[3]])
            ps3 = PS[3].ap.rearrange("p (a n) -> p a n", a=4)
            for (r0, c0) in ((0, 0), (64, 64)):
                for p in range(4):
                    STT("dve", stT3[r0:r0 + 64, p, :], stT3[r0:r0 + 64, p, :], decC.ap[r0:r0 + 64, p:p + 1], ps3[r0:r0 + 64, p, c0:c0 + 64],
                        ALU.mult, ALU.add, R=[stT, decC, PS[3]], W=[stT])
        if own:
            for tt in range(4):
                tsl = slice(og * GS + tt * 128, og * GS + (tt + 1) * 128)
                for half in range(2):
                    ps = PS[6 + half]
                    for ft in range(8):
                        lhs = ym3[:, ft, tsl] if ft < 4 else yr3[:, ft - 4, tt * 128:(tt + 1) * 128]
                        MM(ps.ap[:, :], lhs, wO3[:, ft, half * 512:(half + 1) * 512], start=(ft == 0), stop=(ft == 7),
                           R=[ymT, yrT, wO], W=[ps])
                    tq = t1 if half == 0 else t2
                    hs = slice(half * 512, (half + 1) * 512)
                    TTo("dve", tq.ap[:], ps.ap[:, :], gt1b.ap[:, hs], ALU.mult, R=[ps, gt1b], W=[tq])
                    TTo("pool", xts[tt].ap[:, hs], xts[tt].ap[:, hs], tq.ap[:], ALU.add, R=[xts[tt], tq], W=[xts[tt]])
                fw.dma(x1_d[og * GS + tt * 128: og * GS + (tt + 1) * 128, :], xts[tt].ap[:], reads=[xts[tt]], writes=[X1])
    fw.barrier()
    if stop == "A2":
        for r_ in range(0, TOK, 128):
            fw.dma(out_d[r_:r_ + 128, :], x1_d[r_:r_ + 128, :], reads=[X1], writes=[OUT])
        return finish()

    apos[0] = 0
    wQ = al("wQ", 8 * 2048, BF16)
    Gsb = al("Gsb", PB * 128, BF16)
    x1b = [al("x1b%d" % i, 1024) for i in range(2)]
    xn = al("xnD", 1024, BF16); ss = al("ssD", 4)
    h2T = al("h2T", 8 * PB, BF16)
    qT = al("qT", 16 * PB, BF16)
    s_sb = al("s_sb", 16 * 128); s_tmp = al("s_tmp", 256)
    atop = al("atop", 16 * 16); idxu = al("idxu", 8 * 16, U32)
    cand = al("cand", 8 * 256); candz = cand; cande = al("cande", 8 * 256)
    m16 = al("m16", 16); th = al("th", 8); zc = al("zc", 8)
    tok4 = al("tok4", 4 * 128)
    tT = al("tT", 4 * PB)
    qrep = [al("qrep%d" % i, 8 * 128, BF16) for i in range(2)]
    e_sb = [al("e_sb%d" % i, 128, BF16) for i in range(4)]
    R_sb = [al("R_sb%d" % i, 128, BF16) for i in range(8)]
    OI = [al("OI%d" % i, 128, BF16) for i in range(8)]
    ubuf = [al("ubuf%d" % i, 1024, BF16) for i in range(4)]
    vbuf = [al("vbuf%d" % i, 1024, BF16) for i in range(4)]
    ge = [al("ge%d" % i, PB, BF16) for i in range(4)]
    WT = [al("WT%d" % i, PB, BF16) for i in range(4)]
    x2 = T(cand.ap[:, 0:1024], "x2"); osb = T(cande.ap[:, 0:1024], "osb")
    x2 = cand; osb = cande
    prT = [T(PS[s_ // 4].ap[:, (s_ % 4) * 128:(s_ % 4 + 1) * 128], "pr%d" % s_) for s_ in range(8)]
    fw.dma(wQ.ap.rearrange("p (k n) -> p k n", k=8), w_query.rearrange("(k p) n -> p k n", p=128), reads=[DR], writes=[wQ], q="pool")
    wQ3 = wQ.ap.rearrange("p (k n) -> p k n", k=8)
    GSW = PB
    h23 = h2T.ap.rearrange("p (k n) -> p k n", k=8)
    qT3 = qT.ap.rearrange("p (a n) -> p a n", a=16)
    s3 = s_sb.ap.rearrange("p (a n) -> p a n", a=16)
    at3 = atop.ap.rearrange("p (a n) -> p a n", a=16)
    ix3 = idxu.ap.rearrange("p (a n) -> p a n", a=8)
    cand3 = cand.ap.rearrange("p (a n) -> p a n", a=8)
    cz3 = candz.ap.rearrange("p (a n) -> p a n", a=8)
    ce3 = cande.ap.rearrange("p (a n) -> p a n", a=8)
    tk3 = tok4.ap.rearrange("p (a n) -> p a n", a=4)
    tT3 = tT.ap.rearrange("p (a n) -> p a n", a=4)
    G3 = Gsb.ap.rearrange("p (t i) -> p t i", t=PB)
    ti = 0
    for blk in range(NPB):
        for tt in range(2):
            r0 = blk * PB + tt * 128
            fw.dma(x1b[tt].ap[:], x1_d[r0:r0 + 128, :], reads=[X1], writes=[x1b[tt]])
            norm_transpose(x1b[tt], xn, ss, h2T, tt * 128, a2c, sh2c, PS[6])
        for a in range(16):
            ps = PS[4 + a % 2]
            for kc in range(8):
                MM(ps.ap[:, 0:PB], wQ3[:, kc, a * 128:(a + 1) * 128], h23[:, kc, :], start=(kc == 0), stop=(kc == 7), R=[wQ, h2T], W=[ps])
            if a % 2 == 0:
                CP("dve", qT3[:, a, :], ps.ap[:, 0:PB], R=[ps], W=[qT])
            else:
                ACT(qT3[:, a, :], ps.ap[:, 0:PB], AF.Copy, R=[ps], W=[qT])
        for tt in range(2):
            tsl = slice(tt * 128, (tt + 1) * 128)
            for a in range(16):
                ps = PS[a // 4]
                MM(ps.ap[:, (a % 4) * 128:(a % 4 + 1) * 128], qT3[:, a, tsl], skb.ap[:, a % 2, :], R=[qT, skb], W=[ps])
            for b4 in range(4):
                ACT(s_sb.ap[:, b4 * 512:(b4 + 1) * 512], PS[b4].ap[:, :], AF.Copy, R=[PS[b4]], W=[s_sb])
            for a in range(16):
                hh, pp = a // 2, a % 2
                fw.op("dve", lambda e, a=a: e.max(out=at3[:, a, 0:8], in_=s3[:, a, :]), reads=[s_sb], writes=[atop])
                if pp == 0:
                    fw.op("dve", lambda e, a=a, hh=hh: e.max_index(out=ix3[:, hh, 0:8], in_max=at3[:, a, 0:8], in_values=s3[:, a, :]),
                          reads=[s_sb, atop], writes=[idxu])
                fw.op("dve", lambda e, a=a: e.match_replace(out=s_tmp.ap[:, 0:128], in_to_replace=at3[:, a, 0:8], in_values=s3[:, a, :], imm_value=-1e30),
                      reads=[s_sb, atop], writes=[s_tmp])
                fw.op("dve", lambda e, a=a: e.max(out=at3[:, a, 8:16], in_=s_tmp.ap[:, 0:128]), reads=[s_tmp], writes=[atop])
                if pp == 0:
                    fw.op("dve", lambda e, a=a, hh=hh: e.max_index(out=ix3[:, hh, 8:16], in_max=at3[:, a, 8:16], in_values=s_tmp.ap[:, 0:128]),
                          reads=[s_tmp, atop], writes=[idxu])
            for hh in range(8):
                TTo("dve", cand3[:, hh, :].rearrange("p (k l) -> p k l", k=16),
                    at3[:, 2 * hh, :].unsqueeze(2).to_broadcast([128, 16, 16]),
                    at3[:, 2 * hh + 1, :].unsqueeze(1).to_broadcast([128, 16, 16]), ALU.add, R=[atop], W=[cand])
            for hh in range(8):
                fw.op("dve", lambda e, hh=hh: e.max(out=m16.ap[:, 0:8], in_=cand3[:, hh, :]), reads=[cand], writes=[m16])
                fw.op("dve", lambda e, hh=hh: e.match_replace(out=s_tmp.ap[:, 0:256], in_to_replace=m16.ap[:, 0:8], in_values=cand3[:, hh, :], imm_value=-1e30),
                      reads=[cand, m16], writes=[s_tmp])
                fw.op("dve", lambda e: e.max(out=m16.ap[:, 8:16], in_=s_tmp.ap[:, 0:256]), reads=[s_tmp], writes=[m16])
                CP("dve", th.ap[:, hh:hh + 1], m16.ap[:, 15:16], R=[m16], W=[th])
            th_b = th.ap.unsqueeze(2).to_broadcast([128, 8, 256])
            TTo("dve", cz3, cand3, th_b, ALU.subtract, R=[cand, th], W=[candz])
            ACT(cande.ap[:], candz.ap[:], AF.Exp, R=[candz], W=[cande])
            STT("dve", ce3, cz3, 0.0, ce3, ALU.is_ge, ALU.mult, R=[candz, cande], W=[cande])
            RSUM(zc.ap[:], ce3, R=[cande], W=[zc])
            RECIP(zc.ap[:], zc.ap[:], R=[zc], W=[zc])
            a0v = atop.ap.rearrange("p (h q k) -> p h q k", h=8, q=2)[:, :, 0, :]
            u3 = tk3[:, 0, :].rearrange("p (h k) -> p h k", h=8)
            th16 = th.ap.unsqueeze(2).to_broadcast([128, 8, 16])
            TTo("dve", u3, a0v, th16, ALU.subtract, R=[atop, th], W=[tok4])
            TS("dve", tk3[:, 1, :], tk3[:, 0, :], -1.0, -1e-5, ALU.mult, ALU.add, R=[tok4], W=[tok4])
            CP("dve", tk3[:, 2, :], idxu.ap[:], R=[idxu], W=[tok4])
            CP("dve", tk3[:, 3, :].rearrange("p (h k) -> p h k", h=8), zc.ap.unsqueeze(2).to_broadcast([128, 8, 16]), R=[zc], W=[tok4])
            for a in range(4):
                TR(PS[5].ap[:, a * 128:(a + 1) * 128], tk3[:, a, :], idf.ap[:], R=[tok4, idf], W=[PS[5]])
            CP("dve", tT3[:, :, tsl], PS[5].ap.rearrange("p (a n) -> p a n", a=4), R=[PS[5]], W=[tT])
        LA = 3
        prB = [PS[0], PS[1], PS[4], PS[5], PS[6], PS[7]]
        q1all = qT.ap.rearrange("p (h q n) -> p h q n", h=8, q=2)

        def build_qrep(gi):
            buf = qrep[gi % 2]
            q1 = q1all[:, :, 1, gi * 8:(gi + 1) * 8]
            CP("dve", buf.ap.rearrange("p (t h k) -> p t h k", t=8, h=8),
               q1.rearrange("p h t -> p t h").unsqueeze(3).to_broadcast([128, 8, 8, 16]), R=[qT], W=[buf])

        def g_front(t):
            buf = qrep[(t // 8) % 2]
            qr3 = buf.ap.rearrange("p (t n) -> p t n", t=8)
            pr = prB[t % 6]
            es, rs, oi = e_sb[t % 4], R_sb[t % 8], OI[t % 8]
            MM(pr.ap[:, 0:128], qr3[:, t % 8, :], skb.ap[:, 1, :], R=[buf, skb], W=[pr])
            ACT(es.ap[:], pr.ap[:, 0:128], AF.Exp, R=[pr, tT], W=[es], bias=tT3[:, 0, t:t + 1])
            STT("dve", rs.ap[:], pr.ap[:, 0:128], tT3[:, 1, t:t + 1], es.ap[:], ALU.is_ge, ALU.mult, R=[pr, tT, es], W=[rs])
            TS("dve", oi.ap[:], iotaf.ap[:], tT3[:, 2, t:t + 1], tT3[:, 3, t:t + 1], ALU.is_equal, ALU.mult, R=[iotaf, tT], W=[oi])

        def g_back(t):
            rs, oi = R_sb[t % 8], OI[t % 8]
            pg = PS[2 + (t // 4) % 2]
            MM(pg.ap[:, (t % 4) * 128:(t % 4 + 1) * 128], rs.ap[:], oi.ap[:], R=[rs, oi], W=[pg])
            if t % 4 == 3:
                ACT(G3[:, t - 3:t + 1, :], pg.ap.rearrange("p (t i) -> p t i", t=4), AF.Copy, R=[pg], W=[Gsb])

        build_qrep(0)
        for t in range(PB + LA):
            if t < PB:
                if t % 8 == 0 and t // 8 + 1 < PB // 8:
                    build_qrep(t // 8 + 1)
                g_front(t)
            if t - LA >= 0:
                g_back(t - LA)
        def e_front(i):
            ub, vb = ubuf[i % 4], vbuf[i % 4]
            fw.dma(ub.ap[:], ub_d[i], reads=[UB], writes=[ub])
            fw.dma(vb.ap[:], vb_d[i * 128:(i + 1) * 128, :], reads=[VB], writes=[vb], q="sp")
            pa = PS[i % 4]
            for kc in range(8):
                MM(pa.ap[:, 0:PB], ub.ap[:, kc * 128:(kc + 1) * 128], h23[:, kc, :], start=(kc == 0), stop=(kc == 7), R=[ub, h2T], W=[pa])
            gg, wt = ge[i % 4], WT[i % 4]
            ACT(gg.ap[:], pa.ap[:, 0:PB], AF.Gelu, R=[pa], W=[gg])
            TTo("dve", wt.ap[:], gg.ap[:], G3[:, :, i], ALU.mult, R=[gg, Gsb], W=[wt])

        def e_back(i):
            vb, wt = vbuf[i % 4], WT[i % 4]
            for tt in range(2):
                for half in range(2):
                    po = PS[4 + tt * 2 + half]
                    MM(po.ap[:, :], wt.ap[:, tt * 128:(tt + 1) * 128], vb.ap[:, half * 512:(half + 1) * 512],
                       start=(i == 0), stop=(i == 127), R=[wt, vb], W=[po])

        for i in range(130):
            if i < 128:
                e_front(i)
            if i >= 2:
                e_back(i - 2)
        for tt in range(2):
            for half in range(2):
                po = PS[4 + tt * 2 + half]
                hs = slice(half * 512, (half + 1) * 512)
                TTo("dve", x2.ap[:, hs], po.ap[:, :], gt2b.ap[:, hs], ALU.mult, R=[po, gt2b], W=[x2])
            TTo("dve", x2.ap[:, 0:1024], x2.ap[:, 0:1024], x1b[tt].ap[:], ALU.add, R=[x2, x1b[tt]], W=[x2])
            ACT(osb.ap[:, 0:1024], x2.ap[:, 0:1024], AF.Square, R=[x2], W=[osb, ss], accum_out=ss.ap[:, 0:1])
            rstd(ss, ss.ap[:, 0:1], 1.0 / 1024.0, R=[ss])
            STT("dve", osb.ap[:, 0:1024], x2.ap[:, 0:1024], ss.ap[:, 0:1], gfinb.ap[:], ALU.mult, ALU.mult, R=[x2, ss, gfinb], W=[osb])
            r0 = blk * PB + tt * 128
            fw.dma(out_d[r0:r0 + 128, :], osb.ap[:, 0:1024], reads=[osb], writes=[OUT])
    fw.barrier()
    fw.emit(st)
    st.close()
    return nc


def _consts():
    ident = np.eye(128, dtype=np.float32)
    kk = np.arange(128)
    tri = (kk[:, None] <= kk[None, :]).astype(np.float32)
    iota = np.broadcast_to(np.arange(128, dtype=np.float32)[None, :], (128, 128)).copy()
    rc = np.zeros((128, 12), np.float32)
    r = np.arange(128)
    inv16 = (10000.0 ** (-np.arange(0, 32, 2, dtype=np.float32) / 32.0)).astype(np.float32)
    inv32 = (10000.0 ** (-np.arange(0, 64, 2, dtype=np.float32) / 64.0)).astype(np.float32)
    rc[:, 0] = inv16[r % 16]
    rc[:, 1] = math.pi / 2; rc[:, 2] = 1.0
    rc[:, 3] = 0.0
    rc[:, 4] = np.where((r % 32) < 16, -1.0, 1.0)
    rc[:, 5] = inv32[r % 32]
    rc[:, 6] = math.pi / 2; rc[:, 7] = 1.0
    rc[:, 8] = 0.0
    rc[:, 9] = np.where((r % 64) < 32, -1.0, 1.0)
    gamma = 1.0 - 2.0 ** (-5.0 - np.arange(8, dtype=np.float64))
    i = np.arange(128, dtype=np.float64)
    dq = np.zeros((128, 4, 128), np.float64); dk = np.zeros((128, 4, 128), np.float64); decC = np.zeros((128, 4), np.float64)
    for p in range(4):
        for s in range(2):
            h = 2 * p + s
            dq[s * 64:(s + 1) * 64, p, :] = gamma[h] ** (i + 1.0)
            dk[s * 64:(s + 1) * 64, p, :] = gamma[h] ** (-(i + 1.0)) * (64.0 ** -0.5)
            decC[s * 64:(s + 1) * 64, p] = gamma[h] ** 128.0
    return dict(ident=ident, tri=tri, iota=iota, rconst=rc, dq=dq.reshape(128, 512).astype(np.float32),
                dk=dk.reshape(128, 512).astype(np.float32), decC=decC.astype(np.float32))


def _swap_halves(w, nheads, hd):
    w3 = w.reshape(w.shape[0], nheads, 2, hd // 2)
    return np.ascontiguousarray(w3[:, :, ::-1, :]).reshape(w.shape[0], nheads * hd)


def _prep(inputs):
    f = lambda a: np.ascontiguousarray(np.asarray(a, dtype=np.float32))
    x = f(inputs["x"]); c = f(inputs["c"]); positions = np.ascontiguousarray(np.asarray(inputs["positions"], dtype=np.int32))
    w_in = f(inputs["w_in"])[0]
    ql, kv, kr = w_in[:, 0:256], w_in[:, 256:384], w_in[:, 384:416]
    rq, rk, rv, rg = w_in[:, 416:928], w_in[:, 928:1440], w_in[:, 1440:1952], w_in[:, 1952:2464]
    w_in_ext = np.ascontiguousarray(np.concatenate(
        [ql, kv, kr, _swap_halves(kr, 1, 32), rq, _swap_halves(rq, 8, 64), rk, _swap_halves(rk, 8, 64), rv, rg], axis=1))
    w_uq = f(inputs["w_uq"])[0].reshape(256, 8, 96)
    nope = w_uq[:, :, 0:64].reshape(256, 512); rope = np.ascontiguousarray(w_uq[:, :, 64:96]).reshape(256, 256)
    w_uq_ext = np.ascontiguousarray(np.concatenate([nope, rope, _swap_halves(rope, 8, 32)], axis=1))
    w_ukv = f(inputs["w_ukv"])[0].reshape(128, 8, 128)
    w_ukv_ext = np.ascontiguousarray(np.concatenate([w_ukv[:, :, 0:64].reshape(128, 512), w_ukv[:, :, 64:128].reshape(128, 512)], axis=1))
    eu = f(inputs["expert_u"])[0]
    uT = np.ascontiguousarray(eu.reshape(128, 128, 8, 128).transpose(0, 3, 2, 1)).reshape(128, 128, 1024)
    shared = dict(
        w_ada=f(inputs["w_ada"])[0], b_ada=f(inputs["b_ada"]).reshape(1, 6144),
        g1=f(inputs["g_norm1"]).reshape(1, 1024), g2=f(inputs["g_norm2"]).reshape(1, 1024), gfin=f(inputs["g_final"]).reshape(1, 1024),
        w_in=w_in_ext, w_uq=w_uq_ext, w_ukv=w_ukv_ext,
        gq=np.ascontiguousarray(f(inputs["g_q_norm"]).reshape(2, 128).T), gkv=f(inputs["g_kv_norm"]).reshape(128, 1),
        gret=f(inputs["g_ret_norm"]).reshape(1, 512), w_out=f(inputs["w_out"])[0], w_query=f(inputs["w_query"])[0],
        skT=np.ascontiguousarray(f(inputs["sub_keys"])[0].transpose(0, 2, 1)),
        uT=uT, vv=f(inputs["expert_v"])[0],
    )
    shared.update(_consts())
    in_maps = []
    for core in range(8):
        b, half = core // 2, core % 2
        m = dict(shared)
        m["xo"] = np.ascontiguousarray(x[b, half * TOK:(half + 1) * TOK])
        m["xp"] = np.ascontiguousarray(x[b, 0:TOK])
        m["pos"] = np.ascontiguousarray(np.concatenate([positions[b, 0:TOK], positions[b, half * TOK:(half + 1) * TOK]]).reshape(1, NSLOT))
        m["cvec"] = np.ascontiguousarray(c[b].reshape(8, 128).T)
        fl = np.zeros((128, 2), np.float32)
        fl[:, 0] = float(half); fl[:, 1] = (float(half) - 1.0) * 30000.0
        m["flag"] = fl
        in_maps.append(m)
    return in_maps


_NC = None


def kernel(**inputs):
    global _NC
    in_maps = _prep(inputs)
    if _NC is None:
        _NC = build()
    res = run_bass_kernel_spmd(_NC, in_maps, core_ids=list(range(8)))
    out = np.zeros((4, 8192, 1024), np.float32)
    for core in range(8):
        b, half = core // 2, core % 2
        out[b, half * TOK:(half + 1) * TOK] = res.results[core]["out"]
    return out
```

```python
import math
import os
from contextlib import ExitStack
import numpy as np
import concourse.bass as bass
import concourse.mybir as mybir
from concourse.bass_utils import run_bass_kernel_spmd

F32 = mybir.dt.float32
BF16 = mybir.dt.bfloat16
I32 = mybir.dt.int32
U32 = mybir.dt.uint32
ALU = mybir.AluOpType
AF = mybir.ActivationFunctionType
AX = mybir.AxisListType

EPS = 1e-6
NEG = -30000.0


class T:
    __slots__ = ("ap", "name", "lw", "rd")

    def __init__(self, ap, name=""):
        self.ap = ap
        self.name = name
        self.lw = None
        self.rd = []

    def __getitem__(self, k):
        return self.ap[k]


class FW:
    ENGS = ("pe", "act", "dve", "pool", "sp")

    def __init__(self, nc, n_dma_sems=16):
        self.nc = nc
        self.prog = {e: [] for e in self.ENGS}
        self.cnt = {e: 0 for e in self.ENGS}
        self.waited = {e: {} for e in self.ENGS}
        self.n_dma_sems = n_dma_sems
        self.dma_i = {"sp": 0, "pool": 0, "act": 0}
        self.dma_sem_val = {}
        self.sems = {}
        self.dma_last = {}

    def _need(self, eng, ev, waits):
        if ev is None:
            return
        k, v = ev
        if eng == "pe" and k == "pe":
            return
        if self.waited[eng].get(k, 0) >= v:
            return
        self.waited[eng][k] = v
        waits.append((k, v))

    def _deps(self, eng, reads, writes):
        waits = []
        for t in reads:
            self._need(eng, t.lw, waits)
        for t in writes:
            self._need(eng, t.lw, waits)
            for ev in t.rd:
                self._need(eng, ev, waits)
        m = {}
        for k, v in waits:
            m[k] = max(m.get(k, 0), v)
        return list(m.items())

    def _commit(self, ev, reads, writes):
        for t in reads:
            t.rd.append(ev)
            if len(t.rd) > 48:
                mm = {}
                for k, v in t.rd:
                    mm[k] = max(mm.get(k, 0), v)
                t.rd = list(mm.items())
        for t in writes:
            t.lw = ev
            t.rd = []

    def op(self, eng, fn, reads=(), writes=()):
        waits = self._deps(eng, reads, writes)
        self.cnt[eng] += 1
        ev = (eng, self.cnt[eng])
        self.prog[eng].append((waits, fn, (eng, 1)))
        self._commit(ev, reads, writes)
        return ev

    def dma(self, out, in_, reads=(), writes=(), q="sp", **kw):
        i = self.dma_i[q]
        self.dma_i[q] += 1
        k = "dma_%s_%d" % (q, i % self.n_dma_sems)
        waits = self._deps(q, reads, writes)
        prev = self.dma_last.get(k)
        if prev is not None:
            tmp = []
            self._need(q, prev, tmp)
            waits += tmp
        v = self.dma_sem_val.get(k, 0) + 16
        self.dma_sem_val[k] = v
        ev = (k, v)
        self.dma_last[k] = ev
        self.prog[q].append((waits, lambda e: e.dma_start(out=out, in_=in_, **kw), (k, 16)))
        self._commit(ev, reads, writes)
        return ev

    def barrier(self):
        evs = [(e, self.cnt[e]) for e in ("pe", "act", "dve", "pool") if self.cnt[e] > 0]
        evs += list(self.dma_last.values())
        for eng in self.ENGS:
            waits = []
            for ev in evs:
                if ev[0] == eng:
                    continue
                self._need(eng, ev, waits)
            if waits:
                self.prog[eng].append((waits, None, None))

    def final_wait(self, eng, tiles):
        waits = []
        for t in tiles:
            self._need(eng, t.lw, waits)
        if waits:
            self.prog[eng].append((waits, None, None))

    def emit(self, stack):
        nc = self.nc
        keys = list(self.ENGS)
        for q in ("sp", "pool", "act"):
            for j in range(min(self.n_dma_sems, self.dma_i[q])):
                keys.append("dma_%s_%d" % (q, j))
        for k in keys:
            self.sems[k] = stack.enter_context(nc.semaphore(k))
        block = stack.enter_context(nc.Block())
        sems = self.sems

        def run(eng_name):
            def body(e):
                for waits, fn, inc in self.prog[eng_name]:
                    for k, v in waits:
                        e.wait_ge(sems[k], v)
                    if fn is not None:
                        ins = fn(e)
                        ins.then_inc(sems[inc[0]], inc[1])
            return body

        block.tensor(run("pe"))
        block.scalar(run("act"))
        block.vector(run("dve"))
        block.gpsimd(run("pool"))
        block.sync(run("sp"))


TOK = 4096
NSLOT = 8192
GS = 512
NG = NSLOT // GS
PB = 256
NPB = TOK // PB
C_QL, C_KV, C_KR, C_KRS, C_RQ, C_RQS, C_RK, C_RKS, C_RV, C_RG = 0, 256, 384, 416, 448, 960, 1472, 1984, 2496, 3008
NCOL = 3520
ARENA_WORDS = 45 * 1024


def build(stop=None):
    nc = bass.Bass("TRN2", target_bir_lowering=False)
    st = ExitStack()
    fw = FW(nc)

    def din(name, shape, dt=F32):
        return nc.dram_tensor(name, list(shape), dt, kind="ExternalInput").ap()

    xo = din("xo", [TOK, 1024]); xp = din("xp", [TOK, 1024])
    pos = din("pos", [1, NSLOT], I32)
    cvec = din("cvec", [128, 8])
    w_ada = din("w_ada", [1024, 6144]); b_ada = din("b_ada", [1, 6144])
    g1 = din("g1", [1, 1024]); g2 = din("g2", [1, 1024]); gfin = din("gfin", [1, 1024])
    w_in = din("w_in", [1024, NCOL])
    w_uq = din("w_uq", [256, 1024]); w_ukv = din("w_ukv", [128, 1024])
    gq = din("gq", [128, 2]); gkv = din("gkv", [128, 1]); gret = din("gret", [1, 512])
    w_out = din("w_out", [1024, 1024]); w_query = din("w_query", [1024, 2048])
    skT = din("skT", [2, 128, 128])
    uT = din("uT", [128, 128, 1024]); vv = din("vv", [16384, 1024])
    ident_d = din("ident", [128, 128]); tri_d = din("tri", [128, 128]); iota_d = din("iota", [128, 128])
    rconst_d = din("rconst", [128, 12]); dq_d = din("dq", [128, 512]); dk_d = din("dk", [128, 512])
    decC_d = din("decC", [128, 4]); flag_d = din("flag", [128, 2])
    out_d = nc.dram_tensor("out", [TOK, 1024], F32, kind="ExternalOutput").ap()
    x1_d = nc.dram_tensor("x1s", [TOK, 1024], F32, kind="Internal").ap()
    ub_d = nc.dram_tensor("ubs", [128, 128, 1024], BF16, kind="Internal").ap()
    vb_d = nc.dram_tensor("vbs", [16384, 1024], BF16, kind="Internal").ap()
    DR = T(None, "dram_in")
    dbg_d = nc.dram_tensor("dbg", [128, 16384], F32, kind="ExternalOutput").ap() if stop else None
    DBG = T(dbg_d)
    dbg_pos = [0]
    dbg_stg = []

    def dump(t_, ap, cols):
        if not dbg_stg:
            dbg_stg.append(sbt("dstg", [128, 256]))
        stg = T(dbg_stg[0].ap[:, 0:cols], "stgv")
        stg = dbg_stg[0]
        rows = ap.shape[0]
        MEMSET("dve", stg.ap[:, 0:cols], 0.0, W=[stg])
        CP("dve", stg.ap[0:rows, 0:cols], ap, R=[t_], W=[stg])
        fw.dma(dbg_d[:, dbg_pos[0]:dbg_pos[0] + cols], stg.ap[:, 0:cols], reads=[stg], writes=[DBG])
        print("DBG", t_.name, dbg_pos[0], cols)
        dbg_pos[0] += cols

    def finish():
        fw.barrier()
        fw.emit(st)
        st.close()
        return nc

    X1 = T(x1_d); UB = T(ub_d); VB = T(vb_d); OUT = T(out_d)

    def sbt(name, shape, dt=F32):
        return T(st.enter_context(nc.sbuf_tensor("sb_" + name, list(shape), dt))[:], name)

    arena = st.enter_context(nc.sbuf_tensor("arena", [128, ARENA_WORDS], F32))
    apos = [0]

    def al(name, cols, dt=F32):
        words = (cols * (2 if dt == BF16 else 4) + 3) // 4
        a = arena[:, apos[0]:apos[0] + words]
        apos[0] += words
        assert apos[0] <= ARENA_WORDS, (name, apos[0])
        if dt != F32:
            a = a.bitcast(dt)
        return T(a, name)

    PS = [T(st.enter_context(nc.psum_tensor("ps%d" % i, [128, 512], F32))[:, :], "ps%d" % i) for i in range(8)]

    def MM(out, lhsT, rhs, start=True, stop=True, R=(), W=()):
        fw.op("pe", lambda e: e.matmul(out, lhsT, rhs, start=start, stop=stop), reads=R, writes=W)

    def TR(out, in_, idn, R=(), W=()):
        fw.op("pe", lambda e: e.transpose(out, in_, idn), reads=R, writes=W)

    def ACT(out, in_, func, R=(), W=(), **kw):
        fw.op("act", lambda e: e.activation(out, in_, func, **kw), reads=R, writes=W)

    def TTo(eng, out, in0, in1, op, R=(), W=()):
        fw.op(eng, lambda e: e.tensor_tensor(out, in0, in1, op), reads=R, writes=W)

    def TS(eng, out, in0, s1, s2, op0, op1=None, R=(), W=()):
        if op1 is None:
            fw.op(eng, lambda e: e.tensor_scalar(out, in0, s1, None, op0), reads=R, writes=W)
        else:
            fw.op(eng, lambda e: e.tensor_scalar(out, in0, s1, s2, op0, op1), reads=R, writes=W)

    def STT(eng, out, in0, sc, in1, op0, op1, R=(), W=()):
        fw.op(eng, lambda e: e.scalar_tensor_tensor(out, in0, sc, in1, op0, op1), reads=R, writes=W)

    def CP(eng, out, in_, R=(), W=()):
        fw.op(eng, lambda e: e.tensor_copy(out, in_), reads=R, writes=W)

    def RSUM(out, in_, R=(), W=()):
        fw.op("dve", lambda e: e.reduce_sum(out, in_, AX.X), reads=R, writes=W)

    def RECIP(out, in_, R=(), W=()):
        fw.op("dve", lambda e: e.reciprocal(out, in_), reads=R, writes=W)

    def MEMSET(eng, ap, val, W=()):
        fw.op(eng, lambda e: e.memset(ap, val), writes=W)

    def rstd(out_t, in_ap, scale, R=()):
        o = in_ap
        TS("dve", o, in_ap, scale, EPS, ALU.mult, ALU.add, R=list(R), W=[out_t])
        fw.op("act", lambda e: e.sqrt(o, o), reads=[out_t], writes=[out_t])
        RECIP(o, o, R=[out_t], W=[out_t])

    idf = sbt("idf", [128, 128]); idb = sbt("idb", [128, 128], BF16)
    trib = sbt("trib", [128, 128], BF16); trif = sbt("trif", [128, 128])
    iotab = sbt("iotab", [128, 128], BF16); iotaf = sbt("iotaf", [128, 128])
    rconst = sbt("rconst", [128, 12]); dq = sbt("dq", [128, 512]); dk = sbt("dk", [128, 512])
    decC = sbt("decC", [128, 4]); flag = sbt("flag", [128, 2])
    onesb = sbt("onesb", [128, 128], BF16)
    gt1b = sbt("gt1b", [128, 1024]); gt2b = sbt("gt2b", [128, 1024]); gfinb = sbt("gfinb", [128, 1024])
    gretb = sbt("gretb", [128, 512])
    a1c = sbt("a1c", [128, 8]); sh1c = sbt("sh1c", [128, 8]); a2c = sbt("a2c", [128, 8]); sh2c = sbt("sh2c", [128, 8])
    gqc = sbt("gqc", [128, 2]); gkvc = sbt("gkvc", [128, 1])
    wuq = sbt("wuq", [128, 2, 1024], BF16); wukv = sbt("wukv", [128, 1024], BF16)
    skb = sbt("skb", [128, 2, 128], BF16)

    for t_, d_ in ((idf, ident_d), (trif, tri_d), (iotaf, iota_d), (rconst, rconst_d), (dq, dq_d), (dk, dk_d),
                   (decC, decC_d), (flag, flag_d), (gqc, gq), (gkvc, gkv)):
        fw.dma(t_.ap[:], d_[:, :], reads=[DR], writes=[t_])
    CP("dve", idb.ap[:], idf.ap[:], R=[idf], W=[idb])
    CP("dve", trib.ap[:], trif.ap[:], R=[trif], W=[trib])
    CP("dve", iotab.ap[:], iotaf.ap[:], R=[iotaf], W=[iotab])
    MEMSET("dve", onesb.ap[:], 1.0, W=[onesb])
    fw.dma(gfinb.ap[:], gfin.partition_broadcast(128), reads=[DR], writes=[gfinb])
    fw.dma(gretb.ap[:], gret.partition_broadcast(128), reads=[DR], writes=[gretb])
    fw.dma(wuq.ap[:], w_uq.rearrange("(k p) n -> p k n", p=128), reads=[DR], writes=[wuq], q="pool")
    fw.dma(wukv.ap[:], w_ukv[:, :], reads=[DR], writes=[wukv], q="pool")
    fw.dma(skb.ap[:], skT.rearrange("a p n -> p a n"), reads=[DR], writes=[skb], q="pool")
    for i in (range(0, 128, 8) if 'nocast' not in os.environ.get('KSKIP', '') else []):
        fw.dma(ub_d[i:i + 8].rearrange("i p n -> p i n"), uT[i:i + 8].rearrange("i p n -> p i n"),
               reads=[DR], writes=[UB], q="pool")
        fw.dma(vb_d[i * 128:(i + 8) * 128, :].rearrange("(i p) n -> p i n", p=128),
               vv[i * 128:(i + 8) * 128, :].rearrange("(i p) n -> p i n", p=128), reads=[DR], writes=[VB], q="pool")

    apos[0] = 0
    cv = al("cv", 8); scv = al("scv", 8); lrep = al("lrep", 8 * 128, BF16)
    modbc = al("modbc", 6144); wa = [al("wa0", 3072, BF16), al("wa1", 3072, BF16)]
    tmpa = al("tmpa", 1024); g1bc = al("g1bc", 1024); g2bc = al("g2bc", 1024)
    fw.dma(cv.ap[:], cvec[:, :], reads=[DR], writes=[cv])
    ACT(scv.ap[:], cv.ap[:], AF.Silu, R=[cv], W=[scv])
    CP("dve", lrep.ap.rearrange("p (k m) -> p k m", k=8), scv.ap.unsqueeze(2).to_broadcast([128, 8, 128]), R=[scv], W=[lrep])
    fw.dma(modbc.ap[:], b_ada.partition_broadcast(128), reads=[DR], writes=[modbc])
    fw.dma(g1bc.ap[:], g1.partition_broadcast(128), reads=[DR], writes=[g1bc])
    fw.dma(g2bc.ap[:], g2.partition_broadcast(128), reads=[DR], writes=[g2bc])
    di = 0
    for half in range(2):
        for kc in range(8):
            w = wa[di % 2]; di += 1
            fw.dma(w.ap[:], w_ada[kc * 128:(kc + 1) * 128, half * 3072:(half + 1) * 3072], reads=[DR], writes=[w], q="pool")
            for nt in range(6):
                MM(PS[nt].ap[:, :], lrep.ap[:, kc * 128:(kc + 1) * 128], w.ap[:, nt * 512:(nt + 1) * 512],
                   start=(kc == 0), stop=(kc == 7), R=[lrep, w], W=[PS[nt]])
        for nt in range(6):
            sl = modbc.ap[:, half * 3072 + nt * 512: half * 3072 + (nt + 1) * 512]
            TTo("dve", sl, PS[nt].ap[:, :], sl, ALU.add, R=[PS[nt], modbc], W=[modbc])
    CP("dve", gt1b.ap[:], modbc.ap[:, 2048:3072], R=[modbc], W=[gt1b])
    CP("dve", gt2b.ap[:], modbc.ap[:, 5120:6144], R=[modbc], W=[gt2b])
    idf3 = idf.ap.unsqueeze(1).to_broadcast([128, 8, 128])

    def diag_extract(dst, src_ap, R):
        t3 = tmpa.ap.rearrange("p (k m) -> p k m", k=8)
        TTo("dve", t3, src_ap.rearrange("p (k m) -> p k m", k=8), idf3, ALU.mult, R=list(R) + [idf], W=[tmpa])
        RSUM(dst.ap[:], t3, R=[tmpa], W=[dst])

    for (sc_off, sh_off, gbc, ac, shc) in ((1024, 0, g1bc, a1c, sh1c), (4096, 3072, g2bc, a2c, sh2c)):
        STT("dve", gbc.ap[:], modbc.ap[:, sc_off:sc_off + 1024], 1.0, gbc.ap[:], ALU.add, ALU.mult, R=[modbc, gbc], W=[gbc])
        diag_extract(ac, gbc.ap, [gbc])
        diag_extract(shc, modbc.ap[:, sh_off:sh_off + 1024], [modbc])
    fw.barrier()
    if stop == "S0":
        for t_ in (a1c, sh1c, a2c, sh2c):
            dump(t_, t_.ap[:, 0:8], 8)
        dump(gt1b, gt1b.ap[:, 0:64], 64); dump(gt2b, gt2b.ap[:, 0:64], 64)
        return finish()

    def slot_src(tile_idx):
        if tile_idx < 32:
            return xp[tile_idx * 128:(tile_idx + 1) * 128, :]
        return xo[(tile_idx - 32) * 128:(tile_idx - 31) * 128, :]

    def norm_transpose(xt, xn, ss, hT, col0, acol, shcol, psb):
        junk = xn
        ACT(xn.ap[:], xt.ap[:], AF.Square, R=[xt], W=[xn, ss], accum_out=ss.ap[:, 0:1])
        rstd(ss, ss.ap[:, 0:1], 1.0 / 1024.0, R=[ss])
        ACT(xn.ap[:], xt.ap[:], AF.Copy, R=[xt, ss], W=[xn], scale=ss.ap[:, 0:1])
        pb = psb.ap.bitcast(BF16)
        for kc in range(8):
            TR(pb[:, kc * 128:(kc + 1) * 128], xn.ap[:, kc * 128:(kc + 1) * 128], idb.ap[:], R=[xn, idb], W=[psb])
        for kc in range(8):
            eng = "dve" if kc % 2 == 0 else "pool"
            TS("dve", hT.ap[:, kc * GSW + col0: kc * GSW + col0 + 128], pb[:, kc * 128:(kc + 1) * 128],
               acol.ap[:, kc:kc + 1], shcol.ap[:, kc:kc + 1], ALU.mult, ALU.add, R=[psb, acol, shcol], W=[hT])

    def rope_table(out_t, posf, inv_c, ph_c, sg_c, rows, tmp, tmpi, kf):
        n = GS
        a = tmp.ap[0:rows, 0:n]; ki = tmpi.ap[0:rows, 0:n]; k = kf.ap[0:rows, 0:n]
        TS("dve", a, posf.ap[0:rows, 0:n], rconst.ap[0:rows, inv_c:inv_c + 1], rconst.ap[0:rows, ph_c:ph_c + 1],
           ALU.mult, ALU.add, R=[posf, rconst], W=[tmp])
        TS("dve", k, a, 1.0 / (2.0 * math.pi), None, ALU.mult, R=[tmp], W=[kf])
        CP("dve", ki, k, R=[kf], W=[tmpi])
        CP("dve", k, ki, R=[tmpi], W=[kf])
        STT("dve", a, k, -2.0 * math.pi, a, ALU.mult, ALU.add, R=[kf, tmp], W=[tmp])
        TS("dve", a, a, 3.1415925, -3.1415925, ALU.min, ALU.max, R=[tmp], W=[tmp])
        ACT(a, a, AF.Sin, R=[tmp], W=[tmp])
        TS("dve", out_t.ap[0:rows, 0:n], a, rconst.ap[0:rows, sg_c:sg_c + 1], None, ALU.mult, R=[tmp, rconst], W=[out_t])

    apos[0] = 0
    ymT = al("ymT", 4 * TOK, BF16)
    mark_Y = apos[0]
    qnT = al("qnT", 2 * TOK, BF16)
    kvnT = al("kvnT", NSLOT, BF16)
    KT = al("KT", NSLOT, BF16)
    CQ = al("CQ", TOK, BF16); SQ = al("SQ", TOK, BF16)
    mark_A1 = apos[0]
    GSW = GS
    wA = al("wA", 8 * 448, BF16)
    xts = [al("xtA%d" % i, 1024) for i in range(2)]
    xn = al("xn", 1024, BF16); ss = al("ss", 4)
    hT = al("hT", 8 * GS, BF16)
    posi = al("posi", GS, I32); posf = al("posf", GS)
    tmpt = al("tmpt", GS); mC = al("mC", GS); mS = al("mS", GS); tmpi = al("tmpi", GS, I32); kfl = al("kfl", GS)
    sqb = al("sqb", 2 * GS, BF16); rbc = al("rbc", GS); t1 = al("t1", GS); t2 = al("t2", GS)
    fw.dma(wA.ap.rearrange("p (k n) -> p k n", k=8), w_in[:, 0:448].rearrange("(k p) n -> p k n", p=128),
           reads=[DR], writes=[wA], q="pool")

    def load_pos(g):
        fw.dma(posi.ap[:], pos[:, g * GS:(g + 1) * GS].partition_broadcast(128), reads=[DR], writes=[posi])
        CP("dve", posf.ap[:], posi.ap[:], R=[posi], W=[posf])

    def lat_norm(ps_list, gcol, ncol_div, extra_scale, dst_aps, dstT):
        n = len(ps_list)
        for i, p in enumerate(ps_list):
            ACT(sqb.ap[:, i * GS:(i + 1) * GS], p.ap[:, :], AF.Square, R=[p], W=[sqb])
        for i in range(n):
            MM(PS[7].ap[:, :], onesb.ap[:], sqb.ap[:, i * GS:(i + 1) * GS], start=(i == 0), stop=(i == n - 1), R=[onesb, sqb], W=[PS[7]])
        TS("dve", rbc.ap[:], PS[7].ap[:, :], 1.0 / ncol_div, EPS, ALU.mult, ALU.add, R=[PS[7]], W=[rbc])
        fw.op("act", lambda e: e.sqrt(rbc.ap[:], rbc.ap[:]), reads=[rbc], writes=[rbc])
        RECIP(rbc.ap[:], rbc.ap[:], R=[rbc], W=[rbc])
        if extra_scale != 1.0:
            TS("dve", rbc.ap[:], rbc.ap[:], extra_scale, None, ALU.mult, R=[rbc], W=[rbc])
        for i, p in enumerate(ps_list):
            STT("dve", dst_aps[i], p.ap[:, :], gcol.ap[:, i:i + 1], rbc.ap[:], ALU.mult, ALU.mult, R=[p, gcol, rbc], W=[dstT])

    for g in range(NG):
        own = g >= 8
        for tt in range(4):
            xt = xts[tt % 2]
            fw.dma(xt.ap[:], slot_src(g * 4 + tt), reads=[DR], writes=[xt])
            norm_transpose(xt, xn, ss, hT, tt * 128, a1c, sh1c, PS[6])
        load_pos(g)
        rope_table(mC, posf, 0, 1, 2, 32, tmpt, tmpi, kfl)
        rope_table(mS, posf, 0, 3, 4, 32, tmpt, tmpi, kfl)
        wA3 = wA.ap.rearrange("p (k n) -> p k n", k=8)
        hT3 = hT.ap.rearrange("p (k n) -> p k n", k=8)

        def proj(ps, c0, m):
            for kc in range(8):
                MM(ps.ap[0:m, :], wA3[:, kc, c0:c0 + m], hT3[:, kc, :], start=(kc == 0), stop=(kc == 7), R=[wA, hT], W=[ps])
        proj(PS[2], C_KV, 128)
        lat_norm([PS[2]], gkvc, 128.0, 1.0, [kvnT.ap[:, g * GS:(g + 1) * GS]], kvnT)
        proj(PS[3], C_KR, 32); proj(PS[4], C_KRS, 32)
        TTo("dve", t1.ap[0:32, :], PS[3].ap[0:32, :], mC.ap[0:32, :], ALU.mult, R=[PS[3], mC], W=[t1])
        TTo("dve", t2.ap[0:32, :], PS[4].ap[0:32, :], mS.ap[0:32, :], ALU.mult, R=[PS[4], mS], W=[t2])
        TTo("dve", t1.ap[0:32, :], t1.ap[0:32, :], t2.ap[0:32, :], ALU.add, R=[t1, t2], W=[t1])
        ACT(KT.ap[64:96, g * GS:(g + 1) * GS], t1.ap[0:32, :], AF.Copy, R=[t1], W=[KT])
        if own:
            og = g - 8
            proj(PS[0], C_QL, 128); proj(PS[1], C_QL + 128, 128)
            qn3 = qnT.ap.rearrange("p (k n) -> p k n", k=2)
            lat_norm([PS[0], PS[1]], gqc, 256.0, 96.0 ** -0.5,
                     [qn3[:, 0, og * GS:(og + 1) * GS], qn3[:, 1, og * GS:(og + 1) * GS]], qnT)
            CP("dve", CQ.ap[0:32, og * GS:(og + 1) * GS], mC.ap[0:32, :], R=[mC], W=[CQ])
            CP("dve", SQ.ap[0:32, og * GS:(og + 1) * GS], mS.ap[0:32, :], R=[mS], W=[SQ])
    fw.barrier()
    if stop == "A1":
        dump(kvnT, kvnT.ap[:, 0:256], 256); dump(kvnT, kvnT.ap[:, 4096:4352], 256)
        dump(qnT, qnT.ap[:, 0:256], 256); dump(qnT, qnT.ap[:, 4096:4352], 256)
        dump(KT, KT.ap[64:96, 0:256], 256); dump(KT, KT.ap[64:96, 4096 + 3840:8192], 256)
        return finish()

    apos[0] = mark_A1
    t1 = al("t1B", GS); t2 = al("t2B", GS)
    Vaug = [al("Vaug0", 64 * 128, BF16), al("Vaug1", 64 * 128, BF16)]
    QT = al("QT", TOK, BF16)
    PT = [al("PT%d" % i, 512, BF16) for i in range(4)]
    lsb = al("lsb", 512)
    kb_col = flag.ap[:, 1:2]
    MEMSET("dve", Vaug[0].ap[:], 1.0, W=[Vaug[0]])
    MEMSET("pool", Vaug[1].ap[:], 1.0, W=[Vaug[1]])
    ym3 = ymT.ap.rearrange("p (k n) -> p k n", k=4)
    pti = 0
    for h in range(8):
        par = h % 2
        Va = Vaug[par]
        Va3 = Va.ap.rearrange("p (b n) -> p b n", b=64)
        voff = 0 if par == 0 else 64
        for g in range(NG):
            ps = PS[g % 2]
            MM(ps.ap[0:64, :], wukv.ap[:, h * 64:(h + 1) * 64], kvnT.ap[:, g * GS:(g + 1) * GS], R=[wukv, kvnT], W=[ps])
            if g % 2 == 0:
                CP("dve", KT.ap[0:64, g * GS:(g + 1) * GS], ps.ap[0:64, :], R=[ps], W=[KT])
            else:
                ACT(KT.ap[0:64, g * GS:(g + 1) * GS], ps.ap[0:64, :], AF.Copy, R=[ps], W=[KT])
        for b8 in range(8):
            ps = PS[2 + b8 % 2]
            for j in range(8):
                kb = b8 * 8 + j
                MM(ps.ap[:, j * 64:(j + 1) * 64], kvnT.ap[:, kb * 128:(kb + 1) * 128], wukv.ap[:, 512 + h * 64: 512 + (h + 1) * 64],
                   R=[wukv, kvnT], W=[ps])
            CP("dve", Va3[:, b8 * 8:(b8 + 1) * 8, voff:voff + 64], ps.ap.rearrange("p (b n) -> p b n", b=8), R=[ps], W=[Va])
        wq3 = wuq.ap
        for og in range(8):
            sl = slice(og * GS, (og + 1) * GS)
            qn3 = qnT.ap.rearrange("p (k n) -> p k n", k=2)
            for (ps, c0, m) in ((PS[4], h * 64, 64), (PS[5], 512 + h * 32, 32), (PS[6], 768 + h * 32, 32)):
                for kt in range(2):
                    MM(ps.ap[0:m, :], wq3[:, kt, c0:c0 + m], qn3[:, kt, sl], start=(kt == 0), stop=(kt == 1), R=[wuq, qnT], W=[ps])
            ACT(QT.ap[0:64, sl], PS[4].ap[0:64, :], AF.Copy, R=[PS[4]], W=[QT])
            TTo("dve", t1.ap[0:32, :], PS[5].ap[0:32, :], CQ.ap[0:32, sl], ALU.mult, R=[PS[5], CQ], W=[t1])
            TTo("dve", t2.ap[0:32, :], PS[6].ap[0:32, :], SQ.ap[0:32, sl], ALU.mult, R=[PS[6], SQ], W=[t2])
            TTo("dve", t1.ap[0:32, :], t1.ap[0:32, :], t2.ap[0:32, :], ALU.add, R=[t1, t2], W=[t1])
            ACT(QT.ap[64:96, sl], t1.ap[0:32, :], AF.Copy, R=[t1], W=[QT])
        for og in range(8):
            qsl0 = og * GS
            nkb = 32 + 4 * (og + 1)
            po = PS[6 + og % 2]

            def att_front(kb):
                ps = PS[kb % 2]
                j = kb - 32 - 4 * og
                c0 = 128 * j if j > 0 else 0
                MM(ps.ap[:, c0:512], KT.ap[0:96, kb * 128:(kb + 1) * 128], QT.ap[0:96, qsl0 + c0: qsl0 + 512], R=[KT, QT], W=[ps])
                pt = PT[kb % 4]
                if kb < 32:
                    ACT(pt.ap[:, c0:512], ps.ap[:, c0:512], AF.Exp, R=[ps, flag], W=[pt], bias=kb_col)
                else:
                    ACT(pt.ap[:, c0:512], ps.ap[:, c0:512], AF.Exp, R=[ps], W=[pt])
                if j >= 0:
                    TTo("dve", pt.ap[:, c0:c0 + 128], pt.ap[:, c0:c0 + 128], trib.ap[:], ALU.mult, R=[pt, trib], W=[pt])
                return (kb, pt, c0)

            def att_back(item):
                kb, pt, c0 = item
                MM(po.ap[:, c0:512], Va3[:, kb, :], pt.ap[:, c0:512], start=(kb == 0), stop=(kb == nkb - 1), R=[Va, pt], W=[po])

            pend = None
            for kb in range(nkb):
                cur = att_front(kb)
                if pend is not None:
                    att_back(pend)
                pend = cur
            att_back(pend)
            osl = slice(qsl0, qsl0 + 512)
            if par == 0:
                ACT(lsb.ap[0:64, :], po.ap[64:128, :], AF.Copy, R=[po], W=[lsb])
                RECIP(lsb.ap[0:64, :], lsb.ap[0:64, :], R=[lsb], W=[lsb])
                TTo("dve", ym3[0:64, h // 2, osl], po.ap[0:64, :], lsb.ap[0:64, :], ALU.mult, R=[po, lsb], W=[ymT])
            else:
                ACT(lsb.ap[64:128, :], po.ap[0:64, :], AF.Copy, R=[po], W=[lsb])
                RECIP(lsb.ap[64:128, :], lsb.ap[64:128, :], R=[lsb], W=[lsb])
                TTo("dve", ym3[64:128, h // 2, osl], po.ap[64:128, :], lsb.ap[64:128, :], ALU.mult, R=[po, lsb], W=[ymT])
    fw.barrier()
    if stop == "B":
        for pr_ in range(4):
            dump(ymT, ym3[:, pr_, 0:256], 256); dump(ymT, ym3[:, pr_, 3840:4096], 256)
        return finish()

    apos[0] = mark_Y
    NR = 3072
    wR = al("wR", 8 * NR, BF16)
    wO = al("wO", 8 * 1024, BF16)
    xts = [al("xtR%d" % i, 1024) for i in range(4)]
    xn = al("xn2", 1024, BF16); ss = al("ss2", 4)
    hT = al("hT2", 8 * GS, BF16)
    posi = al("posi2", GS, I32); posf = al("posf2", GS)
    tmpt = al("tmpt2", GS); rC = al("rC", GS); rS = al("rS", GS); tmpi = al("tmpi2", GS, I32); kfl = al("kfl2", GS)
    t1 = al("t1b", GS); t2 = al("t2b", GS)
    QrT = al("QrT", 4 * GS, BF16); KrT = al("KrT", 4 * GS, BF16)
    Ktok = al("Ktok", 4 * 512, BF16); Vtok = al("Vtok", 4 * 512, BF16); gate = al("gate", 4 * 512, BF16)
    Sm = al("Sm", 8 * 128, BF16); ysb = al("ysb", 512); ysq = al("ysq", 512); yg = al("yg", 512, BF16)
    yrT = al("yrT", 4 * GS, BF16)
    stT = al("stT", 4 * 64); stbF = al("stbF", 4 * 128, BF16)
    st8 = al("st8", 64)
    fw.dma(wR.ap.rearrange("p (k n) -> p k n", k=8), w_in[:, 448:NCOL].rearrange("(k p) n -> p k n", p=128),
           reads=[DR], writes=[wR], q="pool")
    fw.dma(wO.ap.rearrange("p (k n) -> p k n", k=8), w_out.rearrange("(k p) n -> p k n", p=128), reads=[DR], writes=[wO], q="pool")
    MEMSET("dve", stT.ap[:], 0.0, W=[stT])
    MEMSET("dve", stbF.ap[:], 0.0, W=[stbF])
    wR3 = wR.ap.rearrange("p (k n) -> p k n", k=8)
    wO3 = wO.ap.rearrange("p (k n) -> p k n", k=8)
    hT3 = hT.ap.rearrange("p (k n) -> p k n", k=8)
    stT3 = stT.ap.rearrange("p (a n) -> p a n", a=4)
    sbF3 = stbF.ap.rearrange("p (a n) -> p a n", a=4)
    dq3 = dq.ap.rearrange("p (a n) -> p a n", a=4)
    dk3 = dk.ap.rearrange("p (a n) -> p a n", a=4)
    Qr3 = QrT.ap.rearrange("p (a n) -> p a n", a=4)
    Kr3 = KrT.ap.rearrange("p (a n) -> p a n", a=4)
    Kt3 = Ktok.ap.rearrange("p (c n) -> p c n", c=4)
    Vt3 = Vtok.ap.rearrange("p (c n) -> p c n", c=4)
    gt3 = gate.ap.rearrange("p (c n) -> p c n", c=4)
    yr3 = yrT.ap.rearrange("p (a n) -> p a n", a=4)

    def rope_ret(dst3, c_plain, c_sw, dec3):
        for p in range(4):
            for (ps, c0) in ((PS[0], c_plain + p * 128), (PS[1], c_sw + p * 128)):
                for kc in range(8):
                    MM(ps.ap[:, :], wR3[:, kc, c0 - 448:c0 - 448 + 128], hT3[:, kc, :], start=(kc == 0), stop=(kc == 7), R=[wR, hT], W=[ps])
            TTo("dve", t1.ap[:], PS[0].ap[:, :], rC.ap[:], ALU.mult, R=[PS[0], rC], W=[t1])
            TTo("dve", t2.ap[:], PS[1].ap[:, :], rS.ap[:], ALU.mult, R=[PS[1], rS], W=[t2])
            TTo("dve", t1.ap[:], t1.ap[:], t2.ap[:], ALU.add, R=[t1, t2], W=[t1])
            TTo("dve", dst3[:, p, :].rearrange("p (c i) -> p c i", c=4), t1.ap.rearrange("p (c i) -> p c i", c=4),
                dec3[:, p, :].unsqueeze(1).to_broadcast([128, 4, 128]), ALU.mult, R=[t1], W=[QrT if dst3 is Qr3 else KrT])

    for g in range(NG):
        own = g >= 8
        og = g - 8
        for tt in range(4):
            xt = xts[tt]
            fw.dma(xt.ap[:], slot_src(g * 4 + tt), reads=[DR], writes=[xt])
            norm_transpose(xt, xn, ss, hT, tt * 128, a1c, sh1c, PS[6])
        fw.dma(posi.ap[:], pos[:, g * GS:(g + 1) * GS].partition_broadcast(128), reads=[DR], writes=[posi])
        CP("dve", posf.ap[:], posi.ap[:], R=[posi], W=[posf])
        rope_table(rC, posf, 5, 6, 7, 128, tmpt, tmpi, kfl)
        rope_table(rS, posf, 5, 8, 9, 128, tmpt, tmpi, kfl)
        rope_ret(Kr3, C_RK, C_RKS, dk3)
        if own:
            rope_ret(Qr3, C_RQ, C_RQS, dq3)
        for c in range(4):
            pb = PS[2].ap.bitcast(BF16)
            for p in range(4):
                TR(pb[:, p * 128:(p + 1) * 128], Kr3[:, p, c * 128:(c + 1) * 128], idb.ap[:], R=[KrT, idb], W=[PS[2]])
            CP("dve", Kt3[:, c, :], pb[:, 0:512], R=[PS[2]], W=[Ktok])
            for kc in range(8):
                MM(PS[3].ap[:, :], hT3[:, kc, c * 128:(c + 1) * 128], wR3[:, kc, C_RV - 448:C_RV - 448 + 512],
                   start=(kc == 0), stop=(kc == 7), R=[wR, hT], W=[PS[3]])
            ACT(Vt3[:, c, :], PS[3].ap[:, :], AF.Copy, R=[PS[3]], W=[Vtok])
            if own:
                for kc in range(8):
                    MM(PS[4].ap[:, :], hT3[:, kc, c * 128:(c + 1) * 128], wR3[:, kc, C_RG - 448:C_RG - 448 + 512],
                       start=(kc == 0), stop=(kc == 7), R=[wR, hT], W=[PS[4]])
                ACT(gt3[:, c, :], PS[4].ap[:, :], AF.Silu, R=[PS[4]], W=[gate])
        if g == 8:
            TS("dve", stT.ap[:], stT.ap[:], flag.ap[:, 0:1], None, ALU.mult, R=[stT, flag], W=[stT])
        for c in range(4):
            csl = slice(c * 128, (c + 1) * 128)
            if own:
                for r0 in (0, 64):
                    TTo("dve", sbF3[r0:r0 + 64, :, r0:r0 + 64], stT3[r0:r0 + 64, :, :],
                        decC.ap[r0:r0 + 64, :].unsqueeze(2).to_broadcast([64, 4, 64]), ALU.mult, R=[stT, decC], W=[stbF])
                for h in range(8):
                    p, r0 = h // 2, (h % 2) * 64
                    ps = PS[h % 2]
                    MM(ps.ap[:, p * 128:(p + 1) * 128], Kr3[r0:r0 + 64, p, csl], Qr3[r0:r0 + 64, p, csl], R=[KrT, QrT], W=[ps])
                tri3 = trif.ap.unsqueeze(1).to_broadcast([128, 4, 128])
                for hh in range(2):
                    TTo("dve", Sm.ap[:, hh * 512:(hh + 1) * 512].rearrange("p (a n) -> p a n", a=4),
                        PS[hh].ap.rearrange("p (a n) -> p a n", a=4), tri3, ALU.mult, R=[PS[hh], trif], W=[Sm])
                for p in range(4):
                    MM(PS[5].ap[:, p * 128:(p + 1) * 128], Qr3[:, p, csl], sbF3[:, p, :], start=True, stop=False, R=[QrT, stbF], W=[PS[5]])
                    for s2 in range(2):
                        h = 2 * p + s2
                        smb = s2 * 4 + p
                        MM(PS[5].ap[:, h * 64:(h + 1) * 64], Sm.ap[:, smb * 128:(smb + 1) * 128], Vt3[:, c, h * 64:(h + 1) * 64],
                           start=False, stop=True, R=[Sm, Vtok], W=[PS[5]])
                ACT(ysb.ap[:], PS[5].ap[:, :], AF.Copy, R=[PS[5]], W=[ysb])
                ACT(ysq.ap[:], PS[5].ap[:, :], AF.Square, R=[PS[5]], W=[ysq])
                y3 = ysb.ap.rearrange("p (h n) -> p h n", h=8)
                q3 = ysq.ap.rearrange("p (h n) -> p h n", h=8)
                RSUM(st8.ap[:, 0:8], y3, R=[ysb], W=[st8])
                RSUM(st8.ap[:, 8:16], q3, R=[ysq], W=[st8])
                TS("dve", st8.ap[:, 0:8], st8.ap[:, 0:8], 1.0 / 64.0, None, ALU.mult, R=[st8], W=[st8])
                TTo("dve", st8.ap[:, 16:24], st8.ap[:, 0:8], st8.ap[:, 0:8], ALU.mult, R=[st8], W=[st8])
                STT("dve", st8.ap[:, 8:16], st8.ap[:, 8:16], 1.0 / 64.0, st8.ap[:, 16:24], ALU.mult, ALU.subtract, R=[st8], W=[st8])
                TS("dve", st8.ap[:, 8:16], st8.ap[:, 8:16], EPS, None, ALU.add, R=[st8], W=[st8])
                fw.op("act", lambda e: e.sqrt(st8.ap[:, 8:16], st8.ap[:, 8:16]), reads=[st8], writes=[st8])
                RECIP(st8.ap[:, 8:16], st8.ap[:, 8:16], R=[st8], W=[st8])
                TTo("dve", y3, y3, st8.ap[:, 0:8].unsqueeze(2).to_broadcast([128, 8, 64]), ALU.subtract, R=[ysb, st8], W=[ysb])
                TTo("dve", y3, y3, st8.ap[:, 8:16].unsqueeze(2).to_broadcast([128, 8, 64]), ALU.mult, R=[ysb, st8], W=[ysb])
                TTo("dve", ysb.ap[:], ysb.ap[:], gretb.ap[:], ALU.mult, R=[ysb, gretb], W=[ysb])
                TTo("dve", yg.ap[:], ysb.ap[:], gt3[:, c, :], ALU.mult, R=[ysb, gate], W=[yg])
                pb = PS[2].ap.bitcast(BF16)
                for p in range(4):
                    TR(pb[:, p * 128:(p + 1) * 128], yg.ap[:, p * 128:(p + 1) * 128], idb.ap[:], R=[yg, idb], W=[PS[2]])
                CP("dve", yr3[:, :, csl], pb[:, 0:512].rearrange("p (a n) -> p a n", a=4), R=[PS[2]], W=[yrT])
            for p in range(4):
                MM(PS[3].ap[:, p * 128:(p + 1) * 128], Kt3[:, c, p * 128:(p + 1) * 128], Vt3[:, c, p * 128:(p + 1) * 128], R=[Ktok, Vtok], W=[PS[3]])
            ps3 = PS[3].ap.rearrange("p (a n) -> p a n", a=4)
            for (r0, c0) in ((0, 0), (64, 64)):
                for p in range(4):
                    STT("dve", stT3[r0:r0 + 64, p, :], stT3[r0:r0 + 64, p, :], decC.ap[r0:r0 + 64, p:p + 1], ps3[r0:r0 + 64, p, c0:c0 + 64],
                        ALU.mult, ALU.add, R=[stT, decC, PS[3]], W=[stT])
        if own:
            for tt in range(4):
                tsl = slice(og * GS + tt * 128, og * GS + (tt + 1) * 128)
                for half in range(2):
                    ps = PS[6 + half]
                    for ft in range(8):
                        lhs = ym3[:, ft, tsl] if ft < 4 else yr3[:, ft - 4, tt * 128:(tt + 1) * 128]
                        MM(ps.ap[:, :], lhs, wO3[:, ft, half * 512:(half + 1) * 512], start=(ft == 0), stop=(ft == 7),
                           R=[ymT, yrT, wO], W=[ps])
                    tq = t1 if half == 0 else t2
                    hs = slice(half * 512, (half + 1) * 512)
                    TTo("dve", tq.ap[:], ps.ap[:, :], gt1b.ap[:, hs], ALU.mult, R=[ps, gt1b], W=[tq])
                    TTo("dve", xts[tt].ap[:, hs], xts[tt].ap[:, hs], tq.ap[:], ALU.add, R=[xts[tt], tq], W=[xts[tt]])
                fw.dma(x1_d[og * GS + tt * 128: og * GS + (tt + 1) * 128, :], xts[tt].ap[:], reads=[xts[tt]], writes=[X1])
    fw.barrier()
    if stop == "A2":
        for r_ in range(0, TOK, 128):
            fw.dma(out_d[r_:r_ + 128, :], x1_d[r_:r_ + 128, :], reads=[X1], writes=[OUT])
        return finish()

    apos[0] = 0
    wQ = al("wQ", 8 * 2048, BF16)
    Gsb = al("Gsb", PB * 128, BF16)
    x1b = [al("x1b%d" % i, 1024) for i in range(2)]
    xn = al("xnD", 1024, BF16); ss = al("ssD", 4)
    h2T = al("h2T", 8 * PB, BF16)
    qT = al("qT", 16 * PB, BF16)
    s_sb = al("s_sb", 16 * 128); s_tmp = al("s_tmp", 256)
    atop = al("atop", 16 * 16); idxu = al("idxu", 8 * 16, U32)
    cand = al("cand", 8 * 256); candz = cand; cande = al("cande", 8 * 256)
    m16 = al("m16", 16); th = al("th", 8); zc = al("zc", 8)
    tok4 = al("tok4", 4 * 128)
    tT = al("tT", 4 * PB)
    qrep = [al("qrep%d" % i, 8 * 128, BF16) for i in range(2)]
    e_sb = [al("e_sb%d" % i, 128, BF16) for i in range(4)]
    R_sb = [al("R_sb%d" % i, 128, BF16) for i in range(8)]
    OI = [al("OI%d" % i, 128, BF16) for i in range(8)]
    ubuf = [al("ubuf%d" % i, 1024, BF16) for i in range(4)]
    vbuf = [al("vbuf%d" % i, 1024, BF16) for i in range(4)]
    ge = [al("ge%d" % i, PB, BF16) for i in range(2)]
    WT = [al("WT%d" % i, PB, BF16) for i in range(2)]
    x2 = T(cand.ap[:, 0:1024], "x2"); osb = T(cande.ap[:, 0:1024], "osb")
    x2 = cand; osb = cande
    prT = [T(PS[s_ // 4].ap[:, (s_ % 4) * 128:(s_ % 4 + 1) * 128], "pr%d" % s_) for s_ in range(8)]
    fw.dma(wQ.ap.rearrange("p (k n) -> p k n", k=8), w_query.rearrange("(k p) n -> p k n", p=128), reads=[DR], writes=[wQ], q="pool")
    wQ3 = wQ.ap.rearrange("p (k n) -> p k n", k=8)
    GSW = PB
    h23 = h2T.ap.rearrange("p (k n) -> p k n", k=8)
    qT3 = qT.ap.rearrange("p (a n) -> p a n", a=16)
    s3 = s_sb.ap.rearrange("p (a n) -> p a n", a=16)
    at3 = atop.ap.rearrange("p (a n) -> p a n", a=16)
    ix3 = idxu.ap.rearrange("p (a n) -> p a n", a=8)
    cand3 = cand.ap.rearrange("p (a n) -> p a n", a=8)
    cz3 = candz.ap.rearrange("p (a n) -> p a n", a=8)
    ce3 = cande.ap.rearrange("p (a n) -> p a n", a=8)
    tk3 = tok4.ap.rearrange("p (a n) -> p a n", a=4)
    tT3 = tT.ap.rearrange("p (a n) -> p a n", a=4)
    G3 = Gsb.ap.rearrange("p (t i) -> p t i", t=PB)
    ti = 0
    for blk in range(NPB):
        for tt in range(2):
            r0 = blk * PB + tt * 128
            fw.dma(x1b[tt].ap[:], x1_d[r0:r0 + 128, :], reads=[X1], writes=[x1b[tt]])
            norm_transpose(x1b[tt], xn, ss, h2T, tt * 128, a2c, sh2c, PS[6])
        for a in range(16):
            ps = PS[4 + a % 2]
            for kc in range(8):
                MM(ps.ap[:, 0:PB], wQ3[:, kc, a * 128:(a + 1) * 128], h23[:, kc, :], start=(kc == 0), stop=(kc == 7), R=[wQ, h2T], W=[ps])
            if a % 2 == 0:
                CP("dve", qT3[:, a, :], ps.ap[:, 0:PB], R=[ps], W=[qT])
            else:
                ACT(qT3[:, a, :], ps.ap[:, 0:PB], AF.Copy, R=[ps], W=[qT])
        for tt in range(2):
            tsl = slice(tt * 128, (tt + 1) * 128)
            for a in range(16):
                ps = PS[a // 4]
                MM(ps.ap[:, (a % 4) * 128:(a % 4 + 1) * 128], qT3[:, a, tsl], skb.ap[:, a % 2, :], R=[qT, skb], W=[ps])
            for b4 in range(4):
                ACT(s_sb.ap[:, b4 * 512:(b4 + 1) * 512], PS[b4].ap[:, :], AF.Copy, R=[PS[b4]], W=[s_sb])
            for a in range(16):
                hh, pp = a // 2, a % 2
                fw.op("dve", lambda e, a=a: e.max(out=at3[:, a, 0:8], in_=s3[:, a, :]), reads=[s_sb], writes=[atop])
                if pp == 0:
                    fw.op("dve", lambda e, a=a, hh=hh: e.max_index(out=ix3[:, hh, 0:8], in_max=at3[:, a, 0:8], in_values=s3[:, a, :]),
                          reads=[s_sb, atop], writes=[idxu])
                fw.op("dve", lambda e, a=a: e.match_replace(out=s_tmp.ap[:, 0:128], in_to_replace=at3[:, a, 0:8], in_values=s3[:, a, :], imm_value=-1e30),
                      reads=[s_sb, atop], writes=[s_tmp])
                fw.op("dve", lambda e, a=a: e.max(out=at3[:, a, 8:16], in_=s_tmp.ap[:, 0:128]), reads=[s_tmp], writes=[atop])
                if pp == 0:
                    fw.op("dve", lambda e, a=a, hh=hh: e.max_index(out=ix3[:, hh, 8:16], in_max=at3[:, a, 8:16], in_values=s_tmp.ap[:, 0:128]),
                          reads=[s_tmp, atop], writes=[idxu])
            for hh in range(8):
                TTo("dve", cand3[:, hh, :].rearrange("p (k l) -> p k l", k=16),
                    at3[:, 2 * hh, :].unsqueeze(2).to_broadcast([128, 16, 16]),
                    at3[:, 2 * hh + 1, :].unsqueeze(1).to_broadcast([128, 16, 16]), ALU.add, R=[atop], W=[cand])
            for hh in range(8):
                fw.op("dve", lambda e, hh=hh: e.max(out=m16.ap[:, 0:8], in_=cand3[:, hh, :]), reads=[cand], writes=[m16])
                fw.op("dve", lambda e, hh=hh: e.match_replace(out=s_tmp.ap[:, 0:256], in_to_replace=m16.ap[:, 0:8], in_values=cand3[:, hh, :], imm_value=-1e30),
                      reads=[cand, m16], writes=[s_tmp])
                fw.op("dve", lambda e: e.max(out=m16.ap[:, 8:16], in_=s_tmp.ap[:, 0:256]), reads=[s_tmp], writes=[m16])
                CP("dve", th.ap[:, hh:hh + 1], m16.ap[:, 15:16], R=[m16], W=[th])
            th_b = th.ap.unsqueeze(2).to_broadcast([128, 8, 256])
            TTo("dve", cz3, cand3, th_b, ALU.subtract, R=[cand, th], W=[candz])
            ACT(cande.ap[:], candz.ap[:], AF.Exp, R=[candz], W=[cande])
            STT("dve", ce3, cz3, 0.0, ce3, ALU.is_ge, ALU.mult, R=[candz, cande], W=[cande])
            RSUM(zc.ap[:], ce3, R=[cande], W=[zc])
            RECIP(zc.ap[:], zc.ap[:], R=[zc], W=[zc])
            a0v = atop.ap.rearrange("p (h q k) -> p h q k", h=8, q=2)[:, :, 0, :]
            u3 = tk3[:, 0, :].rearrange("p (h k) -> p h k", h=8)
            th16 = th.ap.unsqueeze(2).to_broadcast([128, 8, 16])
            TTo("dve", u3, a0v, th16, ALU.subtract, R=[atop, th], W=[tok4])
            TS("dve", tk3[:, 1, :], tk3[:, 0, :], -1.0, -1e-5, ALU.mult, ALU.add, R=[tok4], W=[tok4])
            CP("dve", tk3[:, 2, :], idxu.ap[:], R=[idxu], W=[tok4])
            CP("dve", tk3[:, 3, :].rearrange("p (h k) -> p h k", h=8), zc.ap.unsqueeze(2).to_broadcast([128, 8, 16]), R=[zc], W=[tok4])
            for a in range(4):
                TR(PS[5].ap[:, a * 128:(a + 1) * 128], tk3[:, a, :], idf.ap[:], R=[tok4, idf], W=[PS[5]])
            CP("dve", tT3[:, :, tsl], PS[5].ap.rearrange("p (a n) -> p a n", a=4), R=[PS[5]], W=[tT])
        LA = 3
        prB = [PS[0], PS[1], PS[4], PS[5], PS[6], PS[7]]
        q1all = qT.ap.rearrange("p (h q n) -> p h q n", h=8, q=2)

        def build_qrep(gi):
            buf = qrep[gi % 2]
            q1 = q1all[:, :, 1, gi * 8:(gi + 1) * 8]
            CP("dve", buf.ap.rearrange("p (t h k) -> p t h k", t=8, h=8),
               q1.rearrange("p h t -> p t h").unsqueeze(3).to_broadcast([128, 8, 8, 16]), R=[qT], W=[buf])

        def g_front(t):
            buf = qrep[(t // 8) % 2]
            qr3 = buf.ap.rearrange("p (t n) -> p t n", t=8)
            pr = prB[t % 6]
            es, rs, oi = e_sb[t % 4], R_sb[t % 8], OI[t % 8]
            MM(pr.ap[:, 0:128], qr3[:, t % 8, :], skb.ap[:, 1, :], R=[buf, skb], W=[pr])
            ACT(es.ap[:], pr.ap[:, 0:128], AF.Exp, R=[pr, tT], W=[es], bias=tT3[:, 0, t:t + 1])
            STT("dve", rs.ap[:], pr.ap[:, 0:128], tT3[:, 1, t:t + 1], es.ap[:], ALU.is_ge, ALU.mult, R=[pr, tT, es], W=[rs])
            TS("dve", oi.ap[:], iotaf.ap[:], tT3[:, 2, t:t + 1], tT3[:, 3, t:t + 1], ALU.is_equal, ALU.mult, R=[iotaf, tT], W=[oi])

        def g_back(t):
            rs, oi = R_sb[t % 8], OI[t % 8]
            pg = PS[2 + (t // 4) % 2]
            MM(pg.ap[:, (t % 4) * 128:(t % 4 + 1) * 128], rs.ap[:], oi.ap[:], R=[rs, oi], W=[pg])
            if t % 4 == 3:
                ACT(G3[:, t - 3:t + 1, :], pg.ap.rearrange("p (t i) -> p t i", t=4), AF.Copy, R=[pg], W=[Gsb])

        build_qrep(0)
        for t in range(PB + LA):
            if t < PB:
                if t % 8 == 0 and t // 8 + 1 < PB // 8:
                    build_qrep(t // 8 + 1)
                g_front(t)
            if t - LA >= 0:
                g_back(t - LA)
        def e_front(i):
            ub, vb = ubuf[i % 4], vbuf[i % 4]
            fw.dma(ub.ap[:], ub_d[i], reads=[UB], writes=[ub])
            fw.dma(vb.ap[:], vb_d[i * 128:(i + 1) * 128, :], reads=[VB], writes=[vb], q="sp")
            pa = PS[i % 2]
            for kc in range(8):
                MM(pa.ap[:, 0:PB], ub.ap[:, kc * 128:(kc + 1) * 128], h23[:, kc, :], start=(kc == 0), stop=(kc == 7), R=[ub, h2T], W=[pa])
            gg, wt = ge[i % 2], WT[i % 2]
            ACT(gg.ap[:], pa.ap[:, 0:PB], AF.Gelu, R=[pa], W=[gg])
            TTo("dve", wt.ap[:], gg.ap[:], G3[:, :, i], ALU.mult, R=[gg, Gsb], W=[wt])

        def e_back(i):
            vb, wt = vbuf[i % 4], WT[i % 2]
            for tt in range(2):
                for half in range(2):
                    po = PS[4 + tt * 2 + half]
                    MM(po.ap[:, :], wt.ap[:, tt * 128:(tt + 1) * 128], vb.ap[:, half * 512:(half + 1) * 512],
                       start=(i == 0), stop=(i == 127), R=[wt, vb], W=[po])

        for i in range(129):
            if i < 128:
                e_front(i)
            if i >= 1:
                e_back(i - 1)
        for tt in range(2):
            for half in range(2):
                po = PS[4 + tt * 2 + half]
                hs = slice(half * 512, (half + 1) * 512)
                TTo("dve", x2.ap[:, hs], po.ap[:, :], gt2b.ap[:, hs], ALU.mult, R=[po, gt2b], W=[x2])
            TTo("dve", x2.ap[:, 0:1024], x2.ap[:, 0:1024], x1b[tt].ap[:], ALU.add, R=[x2, x1b[tt]], W=[x2])
            ACT(osb.ap[:, 0:1024], x2.ap[:, 0:1024], AF.Square, R=[x2], W=[osb, ss], accum_out=ss.ap[:, 0:1])
            rstd(ss, ss.ap[:, 0:1], 1.0 / 1024.0, R=[ss])
            STT("dve", osb.ap[:, 0:1024], x2.ap[:, 0:1024], ss.ap[:, 0:1], gfinb.ap[:], ALU.mult, ALU.mult, R=[x2, ss, gfinb], W=[osb])
            r0 = blk * PB + tt * 128
            fw.dma(out_d[r0:r0 + 128, :], osb.ap[:, 0:1024], reads=[osb], writes=[OUT])
    fw.barrier()
    fw.emit(st)
    st.close()
    return nc


def _consts():
    ident = np.eye(128, dtype=np.float32)
    kk = np.arange(128)
    tri = (kk[:, None] <= kk[None, :]).astype(np.float32)
    iota = np.broadcast_to(np.arange(128, dtype=np.float32)[None, :], (128, 128)).copy()
    rc = np.zeros((128, 12), np.float32)
    r = np.arange(128)
    inv16 = (10000.0 ** (-np.arange(0, 32, 2, dtype=np.float32) / 32.0)).astype(np.float32)
    inv32 = (10000.0 ** (-np.arange(0, 64, 2, dtype=np.float32) / 64.0)).astype(np.float32)
    rc[:, 0] = inv16[r % 16]
    rc[:, 1] = math.pi / 2; rc[:, 2] = 1.0
    rc[:, 3] = 0.0
    rc[:, 4] = np.where((r % 32) < 16, -1.0, 1.0)
    rc[:, 5] = inv32[r % 32]
    rc[:, 6] = math.pi / 2; rc[:, 7] = 1.0
    rc[:, 8] = 0.0
    rc[:, 9] = np.where((r % 64) < 32, -1.0, 1.0)
    gamma = 1.0 - 2.0 ** (-5.0 - np.arange(8, dtype=np.float64))
    i = np.arange(128, dtype=np.float64)
    dq = np.zeros((128, 4, 128), np.float64); dk = np.zeros((128, 4, 128), np.float64); decC = np.zeros((128, 4), np.float64)
    for p in range(4):
        for s in range(2):
            h = 2 * p + s
            dq[s * 64:(s + 1) * 64, p, :] = gamma[h] ** (i + 1.0)
            dk[s * 64:(s + 1) * 64, p, :] = gamma[h] ** (-(i + 1.0)) * (64.0 ** -0.5)
            decC[s * 64:(s + 1) * 64, p] = gamma[h] ** 128.0
    return dict(ident=ident, tri=tri, iota=iota, rconst=rc, dq=dq.reshape(128, 512).astype(np.float32),
                dk=dk.reshape(128, 512).astype(np.float32), decC=decC.astype(np.float32))


def _swap_halves(w, nheads, hd):
    w3 = w.reshape(w.shape[0], nheads, 2, hd // 2)
    return np.ascontiguousarray(w3[:, :, ::-1, :]).reshape(w.shape[0], nheads * hd)


def _prep(inputs):
    f = lambda a: np.ascontiguousarray(np.asarray(a, dtype=np.float32))
    x = f(inputs["x"]); c = f(inputs["c"]); positions = np.ascontiguousarray(np.asarray(inputs["positions"], dtype=np.int32))
    w_in = f(inputs["w_in"])[0]
    ql, kv, kr = w_in[:, 0:256], w_in[:, 256:384], w_in[:, 384:416]
    rq, rk, rv, rg = w_in[:, 416:928], w_in[:, 928:1440], w_in[:, 1440:1952], w_in[:, 1952:2464]
    w_in_ext = np.ascontiguousarray(np.concatenate(
        [ql, kv, kr, _swap_halves(kr, 1, 32), rq, _swap_halves(rq, 8, 64), rk, _swap_halves(rk, 8, 64), rv, rg], axis=1))
    w_uq = f(inputs["w_uq"])[0].reshape(256, 8, 96)
    nope = w_uq[:, :, 0:64].reshape(256, 512); rope = np.ascontiguousarray(w_uq[:, :, 64:96]).reshape(256, 256)
    w_uq_ext = np.ascontiguousarray(np.concatenate([nope, rope, _swap_halves(rope, 8, 32)], axis=1))
    w_ukv = f(inputs["w_ukv"])[0].reshape(128, 8, 128)
    w_ukv_ext = np.ascontiguousarray(np.concatenate([w_ukv[:, :, 0:64].reshape(128, 512), w_ukv[:, :, 64:128].reshape(128, 512)], axis=1))
    eu = f(inputs["expert_u"])[0]
    uT = np.ascontiguousarray(eu.reshape(128, 128, 8, 128).transpose(0, 3, 2, 1)).reshape(128, 128, 1024)
    shared = dict(
        w_ada=f(inputs["w_ada"])[0], b_ada=f(inputs["b_ada"]).reshape(1, 6144),
        g1=f(inputs["g_norm1"]).reshape(1, 1024), g2=f(inputs["g_norm2"]).reshape(1, 1024), gfin=f(inputs["g_final"]).reshape(1, 1024),
        w_in=w_in_ext, w_uq=w_uq_ext, w_ukv=w_ukv_ext,
        gq=np.ascontiguousarray(f(inputs["g_q_norm"]).reshape(2, 128).T), gkv=f(inputs["g_kv_norm"]).reshape(128, 1),
        gret=f(inputs["g_ret_norm"]).reshape(1, 512), w_out=f(inputs["w_out"])[0], w_query=f(inputs["w_query"])[0],
        skT=np.ascontiguousarray(f(inputs["sub_keys"])[0].transpose(0, 2, 1)),
        uT=uT, vv=f(inputs["expert_v"])[0],
    )
    shared.update(_consts())
    in_maps = []
    for core in range(8):
        b, half = core // 2, core % 2
        m = dict(shared)
        m["xo"] = np.ascontiguousarray(x[b, half * TOK:(half + 1) * TOK])
        m["xp"] = np.ascontiguousarray(x[b, 0:TOK])
        m["pos"] = np.ascontiguousarray(np.concatenate([positions[b, 0:TOK], positions[b, half * TOK:(half + 1) * TOK]]).reshape(1, NSLOT))
        m["cvec"] = np.ascontiguousarray(c[b].reshape(8, 128).T)
        fl = np.zeros((128, 2), np.float32)
        fl[:, 0] = float(half); fl[:, 1] = (float(half) - 1.0) * 30000.0
        m["flag"] = fl
        in_maps.append(m)
    return in_maps


_NC = None


def kernel(**inputs):
    global _NC
    in_maps = _prep(inputs)
    if _NC is None:
        _NC = build()
    res = run_bass_kernel_spmd(_NC, in_maps, core_ids=list(range(8)))
    out = np.zeros((4, 8192, 1024), np.float32)
    for core in range(8):
        b, half = core // 2, core % 2
        out[b, half * TOK:(half + 1) * TOK] = res.results[core]["out"]
    return out
```
